# Optimizing a Trainium2 kernel written in Bass

```python
import math
import jax, jax.numpy as jnp
from jax import lax
import numpy as np

D_MODEL = 2048
BATCH = 4
SEQ = 4096
DEPTH = 1

HEAD_DIM = 128
ATTN_WIDTH = 3 * D_MODEL // 4
N_ATTN_HEADS = ATTN_WIDTH // HEAD_DIM
GMLP_WIDTH = D_MODEL // 4
GMLP_GROUP_WIDTH = 128
GMLP_GROUPS = GMLP_WIDTH // GMLP_GROUP_WIDTH
GMLP_CHUNK = 128
MIX_WIDTH = ATTN_WIDTH + GMLP_WIDTH
IN_PROJ_WIDTH = 3 * ATTN_WIDTH + 2 * GMLP_WIDTH
DILATED_PAIRS = ((128, 1), (512, 4), (2048, 16))
ATTN_BLOCK = 128
ROPE_THETA = 10000.0

N_EXPERTS = 32
TOP_K = 4
D_FF = D_MODEL
SWIGLU_LIMIT = 7.0
SWIGLU_ALPHA = 1.702
MOE_BLOCK = 128

DEEPNORM_ALPHA = (2 * DEPTH) ** 0.25
DEEPNORM_BETA = (8 * DEPTH) ** -0.25
LN_EPS = 1e-5
NEG_INF = -1e30

kernel_name = "hybrid_dilated_attn_gmlp_moe_deepnorm_adaln"


def layer_norm(x, g=None, b=None):
    xf = x.astype(jnp.float32)
    mu = jnp.mean(xf, axis=-1, keepdims=True)
    xc = xf - mu
    y = xc * lax.rsqrt(jnp.mean(xc * xc, axis=-1, keepdims=True) + LN_EPS)
    if g is not None:
        y = y * g.astype(jnp.float32) + b.astype(jnp.float32)
    return y.astype(x.dtype)


def rope(x, positions):
    half = HEAD_DIM // 2
    inv_freq = jnp.power(ROPE_THETA, -jnp.arange(half, dtype=jnp.float32) * (2.0 / HEAD_DIM))
    ang = positions.astype(jnp.float32)[:, :, None] * inv_freq
    cos = jnp.cos(ang)[:, :, None, :]
    sin = jnp.sin(ang)[:, :, None, :]
    xf = x.astype(jnp.float32)
    x1, x2 = xf[..., :half], xf[..., half:]
    return jnp.concatenate([x1 * cos - x2 * sin, x2 * cos + x1 * sin], axis=-1).astype(x.dtype)


def dilated_branch(q, k, v, window, dil):
    B, H, S, hd = q.shape
    max_rel = window // dil
    assert max_rel <= ATTN_BLOCK
    L = S // dil
    nb = -(-L // ATTN_BLOCK)
    pad = nb * ATTN_BLOCK - L

    def strided(a):
        return a.reshape(B, H, L, dil, hd).transpose(0, 1, 3, 2, 4)

    qs, ks, vs = strided(q), strided(k), strided(v)
    qs = jnp.pad(qs, ((0, 0), (0, 0), (0, 0), (0, pad), (0, 0)))
    kpad = ((0, 0), (0, 0), (0, 0), (ATTN_BLOCK, pad), (0, 0))
    ks = jnp.pad(ks, kpad).reshape(B, H, dil, nb + 1, ATTN_BLOCK, hd)
    vs = jnp.pad(vs, kpad).reshape(B, H, dil, nb + 1, ATTN_BLOCK, hd)
    qb = qs.reshape(B, H, dil, nb, ATTN_BLOCK, hd)
    kw = jnp.concatenate([ks[:, :, :, :-1], ks[:, :, :, 1:]], axis=-2)
    vw = jnp.concatenate([vs[:, :, :, :-1], vs[:, :, :, 1:]], axis=-2)

    logits = jnp.einsum('bhrnqd,bhrnkd->bhrnqk', qb, kw,
                        preferred_element_type=jnp.float32) * (1.0 / math.sqrt(HEAD_DIM))
    qi = jnp.arange(ATTN_BLOCK)[:, None]
    ki = jnp.arange(2 * ATTN_BLOCK)[None, :]
    dist = qi - ki + ATTN_BLOCK
    rel_ok = (dist >= 0) & (dist <= max_rel)
    key_pos = jnp.arange(nb)[:, None, None] * ATTN_BLOCK + ki[None] - ATTN_BLOCK
    mask = rel_ok[None] & (key_pos >= 0)
    logits = jnp.where(mask, logits, NEG_INF)
    lse = jax.nn.logsumexp(logits, axis=-1)
    p = jnp.exp(logits - lse[..., None])
    o = jnp.einsum('bhrnqk,bhrnkd->bhrnqd', p.astype(v.dtype), vw,
                   preferred_element_type=jnp.float32)
    o = o.reshape(B, H, dil, nb * ATTN_BLOCK, hd)[:, :, :, :L]
    o = o.transpose(0, 1, 3, 2, 4).reshape(B, H, S, hd)
    lse = lse.reshape(B, H, dil, nb * ATTN_BLOCK)[:, :, :, :L].transpose(0, 1, 3, 2).reshape(B, H, S)
    return o, lse


def hybrid_mixer(h, positions, w_in, w_spatial, b_spatial, gmlp_ln_g, gmlp_ln_b, w_out):
    B, S, _ = h.shape
    proj = h @ w_in
    A, G = ATTN_WIDTH, GMLP_WIDTH
    q, k, v, u, vg = jnp.split(proj, [A, 2 * A, 3 * A, 3 * A + G], axis=-1)

    q = rope(q.reshape(B, S, N_ATTN_HEADS, HEAD_DIM), positions).transpose(0, 2, 1, 3)
    k = rope(k.reshape(B, S, N_ATTN_HEADS, HEAD_DIM), positions).transpose(0, 2, 1, 3)
    v = v.reshape(B, S, N_ATTN_HEADS, HEAD_DIM).transpose(0, 2, 1, 3)
    outs, lses = zip(*[dilated_branch(q, k, v, w, d) for (w, d) in DILATED_PAIRS])
    mix_w = jax.nn.softmax(jnp.stack(lses), axis=0)
    attn = jnp.einsum('rbhs,rbhsd->bshd', mix_w, jnp.stack(outs)).reshape(B, S, A).astype(h.dtype)

    u = jax.nn.gelu(u, approximate=False)
    vg = layer_norm(jax.nn.gelu(vg, approximate=False), gmlp_ln_g, gmlp_ln_b)
    nc = S // GMLP_CHUNK
    vg = vg.reshape(B, nc, GMLP_CHUNK, GMLP_GROUPS, GMLP_GROUP_WIDTH)
    causal = jnp.tril(jnp.ones((GMLP_CHUNK, GMLP_CHUNK), dtype=bool))
    ws = jnp.where(causal[None], w_spatial, 0.0).astype(vg.dtype)
    spatial = jnp.einsum('gts,bnsgc->bntgc', ws, vg) + b_spatial.T[None, None, :, :, None]
    gm = (u.reshape(B, nc, GMLP_CHUNK, GMLP_GROUPS, GMLP_GROUP_WIDTH) * spatial).reshape(B, S, G)

    return jnp.concatenate([attn, gm.astype(h.dtype)], axis=-1) @ w_out


def moe_ffn(h, router_w, router_b, w_gate_up, b_gate_up, w_down, b_down):
    B, S, D = h.shape
    T = B * S
    xs = h.reshape(T, D)
    logits = (xs @ router_w + router_b).astype(jnp.float32)
    top_logits, top_idx = lax.top_k(logits, TOP_K)
    gates = jax.nn.softmax(top_logits, axis=-1)

    n_assign = T * TOP_K
    expert_of = top_idx.reshape(n_assign)
    token_of = jnp.repeat(jnp.arange(T, dtype=jnp.int32), TOP_K)
    gate_of = gates.reshape(n_assign)
    order = jnp.argsort(expert_of)
    e_sorted = expert_of[order]
    counts = jnp.bincount(expert_of, length=N_EXPERTS)
    starts = jnp.cumsum(counts) - counts
    padded = (counts + MOE_BLOCK - 1) // MOE_BLOCK * MOE_BLOCK
    pad_end = jnp.cumsum(padded)
    pad_start = pad_end - padded
    slot = pad_start[e_sorted] + jnp.arange(n_assign) - starts[e_sorted]
    n_slots = (-(-n_assign // MOE_BLOCK) + N_EXPERTS) * MOE_BLOCK
    n_blocks = n_slots // MOE_BLOCK
    slot_token = jnp.zeros((n_slots,), jnp.int32).at[slot].set(token_of[order])
    slot_gate = jnp.zeros((n_slots,), jnp.float32).at[slot].set(gate_of[order])
    block_expert = jnp.minimum(
        jnp.searchsorted(pad_end, jnp.arange(n_blocks) * MOE_BLOCK, side='right'), N_EXPERTS - 1)
    x_blocks = xs[slot_token].reshape(n_blocks, MOE_BLOCK, D)

    def expert_block(args):
        xb, e = args
        gu = (xb @ w_gate_up[e] + b_gate_up[e]).astype(jnp.float32)
        gate, up = gu[:, :D_FF], gu[:, D_FF:]
        gate = jnp.minimum(gate, SWIGLU_LIMIT)
        up = jnp.clip(up, -SWIGLU_LIMIT, SWIGLU_LIMIT)
        act = ((up + 1.0) * (gate * jax.nn.sigmoid(gate * SWIGLU_ALPHA))).astype(xb.dtype)
        return act @ w_down[e] + b_down[e]

    y_blocks = lax.map(expert_block, (x_blocks, block_expert))
    y = jnp.zeros((T, D), jnp.float32).at[slot_token].add(
        y_blocks.reshape(n_slots, D).astype(jnp.float32) * slot_gate[:, None])
    return y.reshape(B, S, D).astype(h.dtype)


def setup_inputs(seed: int = 0) -> dict:
    key = jax.random.key(seed)
    ks = jax.random.split(key, 22)
    f32 = jnp.float32
    nrm = lambda k, shape, s: jax.random.normal(k, shape, f32) * s
    L = DEPTH
    x = jax.random.normal(ks[0], (BATCH, SEQ, D_MODEL), f32)
    c = jax.random.normal(ks[1], (BATCH, D_MODEL), f32)
    positions = (jnp.arange(SEQ, dtype=jnp.int32)[None, :]
                 + jax.random.randint(ks[2], (BATCH, 1), 0, 1024, dtype=jnp.int32))
    return {
        "x": x,
        "c": c,
        "positions": positions,
        "ada_w": nrm(ks[3], (L, D_MODEL, 6 * D_MODEL), D_MODEL ** -0.5),
        "ada_b": nrm(ks[4], (L, 6 * D_MODEL), 0.02),
        "w_in": nrm(ks[5], (L, D_MODEL, IN_PROJ_WIDTH), D_MODEL ** -0.5),
        "w_spatial": nrm(ks[6], (L, GMLP_GROUPS, GMLP_CHUNK, GMLP_CHUNK), GMLP_CHUNK ** -0.5),
        "b_spatial": 1.0 + nrm(ks[7], (L, GMLP_GROUPS, GMLP_CHUNK), 0.1),
        "gmlp_ln_g": 1.0 + nrm(ks[8], (L, GMLP_WIDTH), 0.05),
        "gmlp_ln_b": nrm(ks[9], (L, GMLP_WIDTH), 0.02),
        "w_out": nrm(ks[10], (L, MIX_WIDTH, D_MODEL), DEEPNORM_BETA * MIX_WIDTH ** -0.5),
        "ln1_g": 1.0 + nrm(ks[11], (L, D_MODEL), 0.05),
        "ln1_b": nrm(ks[12], (L, D_MODEL), 0.02),
        "router_w": nrm(ks[13], (L, D_MODEL, N_EXPERTS), D_MODEL ** -0.5),
        "router_b": nrm(ks[14], (L, N_EXPERTS), 0.01),
        "w_gate_up": nrm(ks[15], (L, N_EXPERTS, D_MODEL, 2 * D_FF), D_MODEL ** -0.5),
        "b_gate_up": nrm(ks[16], (L, N_EXPERTS, 2 * D_FF), 0.01),
        "w_down": nrm(ks[17], (L, N_EXPERTS, D_FF, D_MODEL), DEEPNORM_BETA * D_FF ** -0.5),
        "b_down": nrm(ks[18], (L, N_EXPERTS, D_MODEL), 0.01),
        "ln2_g": 1.0 + nrm(ks[19], (L, D_MODEL), 0.05),
        "ln2_b": nrm(ks[20], (L, D_MODEL), 0.02),
    }


def reference(x, c, positions, ada_w, ada_b, w_in, w_spatial, b_spatial, gmlp_ln_g, gmlp_ln_b,
              w_out, ln1_g, ln1_b, router_w, router_b, w_gate_up, b_gate_up, w_down, b_down,
              ln2_g, ln2_b):
    for l in range(DEPTH):
        mod = (jax.nn.silu(c) @ ada_w[l] + ada_b[l])[:, None, :]
        sh1, sc1, g1, sh2, sc2, g2 = jnp.split(mod, 6, axis=-1)

        h = layer_norm(x) * (1.0 + sc1) + sh1
        mix = hybrid_mixer(h, positions, w_in[l], w_spatial[l], b_spatial[l],
                           gmlp_ln_g[l], gmlp_ln_b[l], w_out[l])
        x = layer_norm(DEEPNORM_ALPHA * x + g1 * mix, ln1_g[l], ln1_b[l])

        h = layer_norm(x) * (1.0 + sc2) + sh2
        ffn = moe_ffn(h, router_w[l], router_b[l], w_gate_up[l], b_gate_up[l], w_down[l], b_down[l])
        x = layer_norm(DEEPNORM_ALPHA * x + g2 * ffn, ln2_g[l], ln2_b[l])
    return x
```

```python
import os
import numpy as np
from contextlib import ExitStack
import concourse.bass as bass
import concourse.mybir as mybir
from concourse.bass_utils import run_bass_kernel_spmd

F32 = mybir.dt.float32
BF16 = mybir.dt.bfloat16
I32 = mybir.dt.int32
AF = mybir.ActivationFunctionType
ALU = mybir.AluOpType
AX = mybir.AxisListType

P = 128
D = 2048
KC = 16
NOWN = 2048
NALL = 4096
NH = 12
HG = 3
NG = NH // HG
NE = 32
CAP = 1024
NSLOT = NE * CAP
ALPHA = float(2.0 ** 0.25)
EPS = 1e-5
BIG = 30000.0
SM_SCALE = float(1.0 / np.sqrt(128.0))
TWO_PI = float(2.0 * np.pi)

C_ID, C_MASK, C_SWAP, C_TRIU, C_USTR, C_EOFF, C_INVF, C_SGN, C_END = 0, 128, 384, 512, 640, 768, 800, 801, 802


def make_consts():
    c = np.zeros((128, C_END), np.float32)
    i = np.arange(128)
    c[:, C_ID:C_ID + 128] = np.eye(128, dtype=np.float32)
    k = i[:, None]
    q = i[None, :]
    c[:, C_MASK:C_MASK + 128] = np.where(k <= q, 0.0, -BIG)
    c[:, C_MASK + 128:C_MASK + 256] = np.where(k >= q, 0.0, -BIG)
    sw = np.zeros((128, 128), np.float32)
    sw[(i + 64) % 128, i] = 1.0
    c[:, C_SWAP:C_SWAP + 128] = sw
    c[:, C_TRIU:C_TRIU + 128] = (k <= q).astype(np.float32)
    c[:, C_USTR:C_USTR + 128] = (k < q).astype(np.float32)
    c[:, C_EOFF:C_EOFF + 32] = (np.arange(32) * CAP)[None, :].astype(np.float32)
    invf = np.power(np.float32(10000.0), -np.arange(64, dtype=np.float32) * np.float32(2.0 / 128)).astype(np.float32)
    c[:, C_INVF] = invf[i % 64]
    c[:, C_SGN] = np.where(i < 64, -1.0, 1.0)
    return c


def sl(start, n, step):
    return slice(start, start + (n - 1) * step + 1, step)


class Buf:
    def __init__(self, t):
        self.t = t
        self.w = {}
        self.r = {}
        self.excl = False

    def __getitem__(self, k):
        return self.t[k]


class Ctx:
    def __init__(self, nc):
        self.nc = nc
        self.E = dict(pe=nc.tensor, act=nc.scalar, dve=nc.vector, pool=nc.gpsimd, sp=nc.sync)
        self.csem = {}
        self.ccnt = {}
        for e in ("pe", "act", "dve", "pool"):
            self.csem[e] = nc.alloc_semaphore(name="c_" + e)
            self.ccnt[e] = 0
        self.waited = {e: {} for e in self.E}
        self.dsems = {}
        self.dcnt = {}
        self.dnext = {}
        for q, n in (("sp", 12), ("pool", 10), ("act", 4)):
            self.dsems[q] = [nc.alloc_semaphore(name="d_%s%d" % (q, i)) for i in range(n)]
            self.dnext[q] = 0
        self.semname = {}

    def _key(self, sem):
        return id(sem)

    def wait_all(self, e, b):
        for t in list(b.w.values()):
            self.wait(e, t)

    def wait(self, e, tok):
        if tok is None:
            return
        sem, val = tok
        if e == "pe" and sem is self.csem["pe"]:
            return
        k = self._key(sem)
        if self.waited[e].get(k, 0) >= val:
            return
        self.E[e].wait_ge(sem, val)
        self.waited[e][k] = val

    @staticmethod
    def _split(reads, writes):
        writes = list(writes) + [b for b in reads if b.excl and b not in writes]
        reads = [b for b in reads if not b.excl]
        return reads, writes

    def _deps(self, e, reads, writes):
        for b in reads:
            for t in list(b.w.values()):
                self.wait(e, t)
        for b in writes:
            for t in list(b.w.values()):
                self.wait(e, t)
            for t in list(b.r.values()):
                self.wait(e, t)

    def _commit(self, tok, reads, writes):
        for b in reads:
            k = self._key(tok[0])
            old = b.r.get(k)
            if old is None or old[1] < tok[1]:
                b.r[k] = tok
        for b in writes:
            k = self._key(tok[0])
            if tok[0] in self.csem.values():
                b.w = {k: tok}
            else:
                b.w[k] = tok
            b.r = {}

    def release(self, bufs):
        for e in ("pe", "act", "dve", "pool", "sp"):
            for b in bufs:
                for t in list(b.w.values()):
                    self.wait(e, t)
                for t in list(b.r.values()):
                    self.wait(e, t)

    def op(self, e, fn, reads=(), writes=()):
        reads, writes = self._split(reads, writes)
        self._deps(e, reads, writes)
        inst = fn(self.E[e])
        self.ccnt[e] += 1
        inst.then_inc(self.csem[e], 1)
        tok = (self.csem[e], self.ccnt[e])
        self._commit(tok, reads, writes)
        return tok

    def pe_group(self, fns, reads=(), writes=()):
        reads, writes = self._split(reads, writes)
        self._deps("pe", reads, writes)
        inst = None
        for fn in fns:
            inst = fn(self.E["pe"])
        self.ccnt["pe"] += 1
        inst.then_inc(self.csem["pe"], 1)
        tok = (self.csem["pe"], self.ccnt["pe"])
        self._commit(tok, reads, writes)
        return tok

    def dma(self, q, fn, reads=(), writes=()):
        sems = self.dsems[q]
        s = sems[self.dnext[q] % len(sems)]
        self.dnext[q] += 1
        k = self._key(s)
        n = self.dcnt.get(k, 0)
        if n > 0:
            self.wait(q, (s, 16 * n))
        self._deps(q, reads, writes)
        inst = fn(self.E[q])
        inst.then_inc(s, 16)
        self.dcnt[k] = n + 1
        tok = (s, 16 * (n + 1))
        self._commit(tok, reads, writes)
        return tok


class Scope:
    def __init__(self, cx):
        self.cx = cx
        self.nc = cx.nc
        self.bufs = []
        self.stack = ExitStack()

    _n = [0]

    def sb(self, name, shape, dt):
        Scope._n[0] += 1
        name = "%s_%d" % (name, Scope._n[0])
        b = Buf(self.stack.enter_context(self.nc.sbuf_tensor(name, list(shape), dt)))
        self.bufs.append(b)
        return b

    def close(self):
        self.cx.release(self.bufs)
        self.stack.close()


def build(stage=99, dbg=False):
    nc = bass.Bass("TRN2", target_bir_lowering=False)
    cx = Ctx(nc)
    op, dma, peg = cx.op, cx.dma, cx.pe_group

    def din(name, shape, dt=F32):
        return Buf(nc.dram_tensor(name, list(shape), dt, kind="ExternalInput").ap())

    def dscr(name, shape, dt, out=False):
        kind = "ExternalOutput" if (out or dbg) else "Internal"
        return Buf(nc.dram_tensor(name, list(shape), dt, kind=kind).ap())

    def sb(name, shape, dt):
        return Buf(nc.alloc_sbuf_tensor(name, list(shape), dt))

    def psum(name, shape, dt=F32):
        b = Buf(nc.alloc_psum_tensor(name, list(shape), dt))
        b.excl = True
        return b

    xin = din("xin", [NALL, D])
    posb = din("posb", [NALL], I32)
    cvec = din("cvec", [P, KC])
    hmask_d = din("hmask", [P, 1])
    consts_d = din("consts", [P, C_END])
    ada_w = din("ada_w", [D, 6 * D])
    ada_b = din("ada_b", [6 * D])
    w_in = din("w_in", [D, 5632])
    hT_scr = dscr("hT_scr", [8, P, KC, 512], BF16)
    mod_scr = dscr("mod_scr", [6, D], F32)
    out_d = dscr("out", [NOWN, D], F32, out=True)

    cst = sb("cst", [P, C_END], F32)
    identb = sb("identb", [P, P], BF16)
    hmask = sb("hmask_s", [P, 1], F32)
    onesb = sb("onesb", [P, P], BF16)
    maskb = sb("maskb", [P, 256], BF16)
    maskh = sb("maskh", [P, P], BF16)
    pswapb = sb("pswapb", [P, P], BF16)
    slot_i = sb("slot_i", [P, 64], I32)
    gates = sb("gates", [P, 16, 4], F32)
    bchk = nc.gpsimd.alloc_register("bchk")
    nc.gpsimd.reg_mov(bchk, NSLOT - 1)
    idxs = [sb("idxs%d" % i, [P, 4], I32) for i in range(2)]
    scR = Scope(cx)
    cosT = scR.sb("cosT", [P, NALL], BF16)
    sinT = scR.sb("sinT", [P, NALL], BF16)
    scA = Scope(cx)
    sc1p = scA.sb("sc1p", [P, D], F32)
    sh1 = scA.sb("sh1", [P, D], F32)

    ps = [psum("ps%d" % i, [P, 512], F32) for i in range(8)]

    dma("sp", lambda q: q.dma_start(out=cst[:, :], in_=consts_d[:, :]), [consts_d], [cst])
    dma("sp", lambda q: q.dma_start(out=hmask[:, :], in_=hmask_d[:, :]), [hmask_d], [hmask])
    op("dve", lambda v: v.tensor_copy(identb[:, :], cst[:, C_ID:C_ID + 128]), [cst], [identb])

    if True:
        sc = Scope(cx)
        posi, ang, ang2 = sc.sb("posi", [P, NALL], I32), sc.sb("angf", [P, NALL], F32), sc.sb("ang2", [P, NALL], F32)
        dma("sp", lambda q: q.dma_start(out=posi[:, :], in_=posb.t.partition_broadcast(P)), [posb], [posi])
        op("dve", lambda v: v.tensor_copy(ang[:, :], posi[:, :]), [posi], [ang])
        op("dve", lambda v: v.tensor_scalar(ang[:, :], ang[:, :], cst[:, C_INVF:C_INVF + 1], None, ALU.mult),
           [ang, cst], [ang])
        op("dve", lambda v: v.tensor_scalar(ang2[:, :], ang[:, :], float(1.0 / TWO_PI), None, ALU.mult), [ang], [ang2])
        op("dve", lambda v: v.tensor_copy(posi[:, :], ang2[:, :]), [ang2], [posi])
        op("dve", lambda v: v.tensor_copy(ang2[:, :], posi[:, :]), [posi], [ang2])
        op("dve", lambda v: v.scalar_tensor_tensor(ang[:, :], ang2[:, :], -TWO_PI, ang[:, :], ALU.mult, ALU.add),
           [ang2, ang], [ang])
        op("dve", lambda v: v.tensor_scalar(ang2[:, :], ang[:, :], float(np.pi), -TWO_PI, ALU.is_gt, ALU.mult), [ang], [ang2])
        op("dve", lambda v: v.tensor_tensor(ang[:, :], ang[:, :], ang2[:, :], ALU.add), [ang, ang2], [ang])
        op("act", lambda a: a.activation(out=ang2[:, :], in_=ang[:, :], func=AF.Sin), [ang], [ang2])
        op("dve", lambda v: v.tensor_scalar(sinT[:, :], ang2[:, :], cst[:, C_SGN:C_SGN + 1], None, ALU.mult),
           [ang2, cst], [sinT])
        op("dve", lambda v: v.tensor_scalar_add(ang[:, :], ang[:, :], float(np.pi / 2)), [ang], [ang])
        op("dve", lambda v: v.tensor_scalar(ang2[:, :], ang[:, :], float(np.pi), -TWO_PI, ALU.is_gt, ALU.mult), [ang], [ang2])
        op("dve", lambda v: v.tensor_tensor(ang[:, :], ang[:, :], ang2[:, :], ALU.add), [ang, ang2], [ang])
        op("act", lambda a: a.activation(out=cosT[:, :], in_=ang[:, :], func=AF.Sin), [ang], [cosT])
        sc.close()

    if True:
        sc = Scope(cx)
        c_t, s_rep, abb, modt = sc.sb("c_t", [P, KC], F32), sc.sb("s_rep", [P, KC, P], F32), sc.sb("abb", [P, D], F32), sc.sb("modt", [P, D], F32)
        aw = [sc.sb("aw%d" % i, [P, D], F32) for i in range(3)]
        dma("sp", lambda q: q.dma_start(out=c_t[:, :], in_=cvec[:, :]), [cvec], [c_t])
        op("act", lambda a: a.activation(out=c_t[:, :], in_=c_t[:, :], func=AF.Silu), [c_t], [c_t])
        for kc in range(KC):
            op("dve", lambda v, kc=kc: v.tensor_copy(s_rep[:, kc, :], c_t[:, kc:kc + 1].to_broadcast([P, P])),
               [c_t], [s_rep])
        nld = 0
        for j in range(6):
            for kc in range(KC):
                b = aw[nld % 3]
                nld += 1
                dma("sp", lambda q, b=b, j=j, kc=kc: q.dma_start(out=b[:, :], in_=ada_w[kc * P:(kc + 1) * P, j * D:(j + 1) * D]),
                    [ada_w], [b])
                peg([lambda pe, b=b, n=n, kc=kc: pe.matmul(ps[n][:, :], s_rep[:, kc, :], b[:, n * 512:(n + 1) * 512],
                                                          start=(kc == 0), stop=(kc == KC - 1)) for n in range(4)],
                    [b, s_rep], [ps[0], ps[1], ps[2], ps[3]])
            dma("sp", lambda q, j=j: q.dma_start(out=abb[:, :], in_=ada_b.t[j * D:(j + 1) * D].partition_broadcast(P)),
                [ada_b], [abb])
            dst = sh1 if j == 0 else (sc1p if j == 1 else modt)
            for n in range(4):
                op("dve", lambda v, n=n, dst=dst: v.tensor_tensor(dst[:, n * 512:(n + 1) * 512], ps[n][:, :],
                                                                 abb[:, n * 512:(n + 1) * 512], ALU.add),
                   [ps[n], abb], [dst])
            if j == 1:
                op("dve", lambda v: v.tensor_scalar_add(sc1p[:, :], sc1p[:, :], 1.0), [sc1p], [sc1p])
            dma("sp", lambda q, j=j, dst=dst: q.dma_start(out=mod_scr[j:j + 1, :], in_=dst[0:1, :]), [dst], [mod_scr])
        sc.close()

    op("dve", lambda v: v.memset(onesb[:, :], 1.0), [], [onesb])
    op("dve", lambda v: v.tensor_copy(maskb[:, :], cst[:, C_MASK:C_MASK + 256]), [cst], [maskb])
    op("dve", lambda v: v.tensor_scalar(maskh[:, :], cst[:, C_MASK + 128:C_MASK + 256], hmask[:, 0:1], None, ALU.add),
       [cst, hmask], [maskh])
    op("dve", lambda v: v.tensor_copy(pswapb[:, :], cst[:, C_SWAP:C_SWAP + 128]), [cst], [pswapb])

    psn = [0]

    def nps():
        b = ps[psn[0] % 8]
        psn[0] += 1
        return b

    def bfview(pb, n=1024):
        return pb[:, :].bitcast(BF16)[:, 0:n]

    def ln_stats(src, st, mv, width=D):
        nchunk = width // 512
        for i in range(nchunk):
            op("dve", lambda v, i=i: v.bn_stats(st[:, i, :], src[:, i * 512:(i + 1) * 512]), [src], [st])
        op("dve", lambda v: v.bn_aggr(mv[:, 0:2], st[:, 0:nchunk, :]), [st], [mv])
        op("dve", lambda v: v.tensor_scalar_add(mv[:, 2:3], mv[:, 1:2], EPS), [mv], [mv])
        op("act", lambda a: a.activation(out=mv[:, 2:3], in_=mv[:, 2:3], func=AF.Sqrt), [mv], [mv])
        op("dve", lambda v: v.reciprocal(mv[:, 2:3], mv[:, 2:3]), [mv], [mv])
        op("dve", lambda v: v.tensor_scalar(mv[:, 3:4], mv[:, 0:1], mv[:, 2:3], -1.0, ALU.mult, ALU.mult), [mv], [mv])

    if True:
        sc = Scope(cx)
        xt = [sc.sb("xt%d" % i, [P, D], F32) for i in range(2)]
        hn = sc.sb("hn", [P, D], F32)
        hb = [sc.sb("hb%d" % i, [P, D], BF16) for i in range(2)]
        hTm = [sc.sb("hTm%d" % i, [P, KC, 512], BF16) for i in range(2)]
        st = sc.sb("st", [P, 4, 6], F32)
        mv = sc.sb("mv", [P, 4], F32)
        for lt in range(32):
            m, sub = lt // 4, lt % 4
            x_ = xt[lt % 2]
            h_ = hb[lt % 2]
            hT_ = hTm[m % 2]
            dma("sp", lambda q, x_=x_, lt=lt: q.dma_start(out=x_[:, :], in_=xin[lt * P:(lt + 1) * P, :]), [xin], [x_])
            ln_stats(x_, st, mv)
            op("act", lambda a, x_=x_: a.activation(out=hn[:, :], in_=x_[:, :], func=AF.Identity, bias=mv[:, 3:4], scale=mv[:, 2:3]),
               [x_, mv], [hn])
            op("pool", lambda g: g.tensor_tensor(hn[:, :], hn[:, :], sc1p[:, :], ALU.mult), [hn, sc1p], [hn])
            op("dve", lambda v, h_=h_: v.tensor_tensor(h_[:, :], hn[:, :], sh1[:, :], ALU.add), [hn, sh1], [h_])
            for half in range(2):
                pb = nps()
                peg([lambda pe, pb=pb, k=k, half=half, h_=h_: pe.transpose(bfview(pb)[:, k * P:(k + 1) * P],
                                                                        h_[:, (half * 8 + k) * P:(half * 8 + k + 1) * P], identb[:, :])
                     for k in range(8)], [h_, identb], [pb])
                op("act", lambda a, pb=pb, half=half, hT_=hT_, sub=sub: a.activation(
                    out=hT_[:, half * 8:(half + 1) * 8, sub * P:(sub + 1) * P],
                    in_=bfview(pb).rearrange("p (k t) -> p k t", t=P), func=AF.Identity), [pb], [hT_])
            if sub == 3:
                dma("sp", lambda q, hT_=hT_, m=m: q.dma_start(out=hT_scr[m, :, :, :], in_=hT_[:, :, :]), [hT_], [hT_scr])
        sc.close()
    scA.close()

    if stage == 1:
        cx.wait_all("sp", hT_scr)
        return nc

    wsT_d = din("wsT", [4, P, P])
    bspT_d = din("bspT", [P, 4])
    gln_g_d = din("gln_g", [512])
    gln_b_d = din("gln_b", [512])
    mixT_scr = dscr("mixT_scr", [4, P, 16, 512], BF16)
    w_in3 = w_in.t.rearrange("(kc p) n -> p kc n", p=P)

    numer_den_scope = None
    for g in range(NG):
        sc = Scope(cx)
        wg = sc.sb("wg", [P, KC, 3 * HG * P], BF16)
        QT = sc.sb("QT", [P, HG, NOWN], BF16)
        KT = sc.sb("KT", [P, HG, NALL], BF16)
        VT = sc.sb("VT", [P, HG, NALL], BF16)
        hTm = [sc.sb("hTg%d" % i, [P, KC, 512], BF16) for i in range(2)]
        rawb = [sc.sb("rawb%d" % i, [P, 512], BF16) for i in range(2)]
        t1 = [sc.sb("t1_%d" % i, [P, 512], F32) for i in range(2)]
        t2 = [sc.sb("t2_%d" % i, [P, 512], F32) for i in range(2)]
        W = HG * P
        for which in range(3):
            c0 = which * 1536 + g * W
            for kc in range(KC):
                dma("pool", lambda q, which=which, c0=c0, kc=kc: q.dma_start(out=wg[:, kc, which * W:(which + 1) * W],
                                                                              in_=w_in[kc * P:(kc + 1) * P, c0:c0 + W]), [w_in], [wg])
        un = 0
        for m in range(8):
            own = m >= 4
            hT_ = hTm[m % 2]
            dma("sp", lambda q, hT_=hT_, m=m: q.dma_start(out=hT_[:, :, :], in_=hT_scr[m, :, :, :]), [hT_scr], [hT_])
            tok0 = m * 512
            for hl in range(HG):
                for which in ((0, 1, 2) if own else (1, 2)):
                    if os.environ.get("KSKIP_QK") and which != 2:
                        continue
                    pb = nps()
                    cb = which * W + hl * P
                    peg([lambda pe, pb=pb, kc=kc, cb=cb, hT_=hT_: pe.matmul(pb[:, :], wg[:, kc, cb:cb + P], hT_[:, kc, :],
                                                                           start=(kc == 0), stop=(kc == KC - 1)) for kc in range(KC)],
                        [wg, hT_], [pb])
                    if which == 2:
                        op("act", lambda a, pb=pb, hl=hl, tok0=tok0: a.activation(out=VT[:, hl, tok0:tok0 + 512], in_=pb[:, :], func=AF.Identity),
                           [pb], [VT])
                        continue
                    rb, a1, a2 = rawb[un % 2], t1[un % 2], t2[un % 2]
                    un += 1
                    QKM = int(os.environ.get("QKM", "9"))
                    if which == 0:
                        dst, dl = QT, tok0 - NOWN
                    else:
                        dst, dl = KT, tok0
                    op("act", lambda a, pb=pb, rb=rb: a.activation(out=rb[:, :], in_=pb[:, :], func=AF.Identity), [pb], [rb])
                    if QKM == 1:
                        op("dve", lambda v, dst=dst, dl=dl, hl=hl, rb=rb: v.tensor_copy(dst[:, hl, dl:dl + 512], rb[:, :]), [rb], [dst])
                        continue
                    pb2 = nps()
                    peg([lambda pe, pb2=pb2, rb=rb: pe.matmul(pb2[:, :], pswapb[:, :], rb[:, :], start=True, stop=True)], [pswapb, rb], [pb2])
                    if QKM == 2:
                        op("dve", lambda v, dst=dst, dl=dl, hl=hl, pb2=pb2: v.tensor_copy(dst[:, hl, dl:dl + 512], pb2[:, :]), [pb2], [dst])
                        continue
                    op("dve", lambda v, pb=pb, a1=a1, tok0=tok0: v.tensor_tensor(a1[:, :], pb[:, :], cosT[:, tok0:tok0 + 512], ALU.mult),
                       [pb, cosT], [a1])
                    if QKM == 3:
                        op("dve", lambda v, dst=dst, dl=dl, hl=hl, a1=a1: v.tensor_copy(dst[:, hl, dl:dl + 512], a1[:, :]), [a1], [dst])
                        continue
                    op("dve", lambda v, pb2=pb2, a2=a2, tok0=tok0: v.tensor_tensor(a2[:, :], pb2[:, :], sinT[:, tok0:tok0 + 512], ALU.mult),
                       [pb2, sinT], [a2])
                    op(os.environ.get("KADD_ENG", "pool"), lambda gp, dst=dst, dl=dl, hl=hl, a1=a1, a2=a2: gp.tensor_tensor(dst[:, hl, dl:dl + 512], a1[:, :], a2[:, :], ALU.add),
                       [a1, a2], [dst])

        if stage in (1.5, 2) and dbg and g == 0:
            dq = dscr("dbg_q", [P, HG, NOWN], BF16)
            dk = dscr("dbg_k", [P, HG, NALL], BF16)
            dv = dscr("dbg_v", [P, HG, NALL], BF16)
            dma("sp", lambda q: q.dma_start(out=dq[:, :, :], in_=QT[:, :, :]), [QT], [dq])
            dma("sp", lambda q: q.dma_start(out=dk[:, :, :], in_=KT[:, :, :]), [KT], [dk])
            dma("sp", lambda q: q.dma_start(out=dv[:, :, :], in_=VT[:, :, :]), [VT], [dv])
            if stage == 1.5:
                for b_ in (dq, dk, dv):
                    cx.wait_all("sp", b_)
                return nc

        sq = sc.sb("sq", [P, NALL], BF16)
        kmx = sc.sb("kmx", [P, 16], F32)
        negb = sc.sb("negb", [1, NOWN], BF16)
        qn = sc.sb("qn", [1, 512], F32)
        numer = sc.sb("numer", [P, NOWN], F32)
        den = sc.sb("den", [P, NOWN], F32)
        attb = sc.sb("attb", [P, NOWN], BF16)
        PT = [sc.sb("PT%d" % i, [P, 256], BF16) for i in range(4)]
        vblk = [sc.sb("vblk%d" % i, [P, P], BF16) for i in range(4)]
        bO = [ps[0], ps[1]]
        bL = [ps[2], ps[3]]
        bS = [ps[4], ps[5]]
        bV = ps[6]
        bX = ps[7]
        for hl in range(HG):
            h = g * HG + hl
            op("act", lambda a, hl=hl: a.activation(out=sq[:, :], in_=KT[:, hl, :], func=AF.Square), [KT], [sq])
            for c in range(8):
                peg([lambda pe, c=c: pe.matmul(bX[:, :], onesb[:, :], sq[:, c * 512:(c + 1) * 512], start=True, stop=True)], [onesb, sq], [bX])
                op("dve", lambda v, c=c: v.tensor_reduce(kmx[:, c:c + 1], bX[:, :], AX.X, ALU.max), [bX], [kmx])
            op("dve", lambda v: v.tensor_reduce(kmx[:, 8:9], kmx[:, 0:8], AX.X, ALU.max), [kmx], [kmx])
            op("act", lambda a: a.activation(out=kmx[:, 9:10], in_=kmx[:, 8:9], func=AF.Sqrt), [kmx], [kmx])
            op("dve", lambda v: v.tensor_scalar(kmx[:, 9:10], kmx[:, 9:10], -1.0, None, ALU.mult), [kmx], [kmx])
            op("act", lambda a, hl=hl: a.activation(out=sq[:, 0:NOWN], in_=QT[:, hl, :], func=AF.Square), [QT], [sq])
            for c in range(4):
                peg([lambda pe, c=c: pe.matmul(bX[:, :], onesb[:, :], sq[:, c * 512:(c + 1) * 512], start=True, stop=True)], [onesb, sq], [bX])
                op("act", lambda a: a.activation(out=qn[0:1, :], in_=bX[0:1, :], func=AF.Sqrt), [bX], [qn])
                op("dve", lambda v, c=c: v.tensor_scalar(negb[0:1, c * 512:(c + 1) * 512], qn[0:1, :], kmx[0:1, 9:10], None, ALU.mult),
                   [qn, kmx], [negb])
            first_branch = True
            sidx = 0
            for d in (1, 4, 16):
                nb_own = 16 // d
                n0 = 16 // d
                for r in range(d):
                    for n in range(n0 - 1, n0 + nb_own):
                        halo = (n == n0 - 1)
                        last = (n == n0 + nb_own - 1)
                        ks = n * P * d + r
                        kAP = KT[:, hl, sl(ks, P, d)]
                        vAP = VT[:, hl, sl(ks, P, d)]
                        if halo:
                            q0, nq = ks + P * d - NOWN, P
                            mAP = maskh[:, :]
                        elif last:
                            q0, nq = ks - NOWN, P
                            mAP = maskb[:, 0:P]
                        else:
                            q0, nq = ks - NOWN, 2 * P
                            mAP = maskb[:, :]
                        qAP = QT[:, hl, sl(q0, nq, d)]
                        nbAP = negb[0:1, sl(q0, nq, d)]
                        S = bS[sidx % 2]
                        pt = PT[sidx % 4]
                        vb = vblk[sidx % 4]
                        sidx += 1
                        peg([lambda pe, S=S, kAP=kAP, qAP=qAP, nq=nq: pe.matmul(S[:, 0:nq], kAP, qAP, start=True, stop=False),
                             lambda pe, S=S, mAP=mAP, nq=nq: pe.matmul(S[:, 0:nq], identb[:, :], mAP, start=False, stop=False),
                             lambda pe, S=S, nbAP=nbAP, nq=nq: pe.matmul(S[:, 0:nq], onesb[0:1, :], nbAP, start=False, stop=True)],
                            [KT, QT, identb, maskb, maskh, onesb, negb], [S])
                        op("act", lambda a, S=S, pt=pt, nq=nq: a.activation(out=pt[:, 0:nq], in_=S[:, 0:nq], func=AF.Exp, scale=SM_SCALE),
                           [S], [pt])
                        peg([lambda pe, vAP=vAP: pe.transpose(bfview(bV)[:, 0:P], vAP, identb[:, :])], [VT, identb], [bV])
                        op("dve", lambda v, vb=vb: v.tensor_copy(vb[:, :], bfview(bV)[:, 0:P]), [bV], [vb])
                        served = []
                        if halo:
                            served.append((n + 1, 0, True))
                        elif last:
                            served.append((n, 0, False))
                        else:
                            served.append((n, 0, False))
                            served.append((n + 1, P, True))
                        for (qb, co, isfirst) in served:
                            O = bO[qb % 2]
                            L = bL[qb % 2]
                            peg([lambda pe, O=O, vb=vb, pt=pt, co=co, isfirst=isfirst: pe.matmul(
                                O[:, 0:P], vb[:, :], pt[:, co:co + P], start=isfirst, stop=(not isfirst))], [vb, pt], [O])
                            peg([lambda pe, L=L, pt=pt, co=co, isfirst=isfirst: pe.matmul(
                                L[:, 0:P], onesb[:, :], pt[:, co:co + P], start=isfirst, stop=(not isfirst))], [onesb, pt], [L])
                            if not isfirst:
                                qs = qb * P * d + r - NOWN
                                nAP = numer[:, sl(qs, P, d)]
                                dAP = den[:, sl(qs, P, d)]
                                if first_branch:
                                    op("dve", lambda v, O=O, nAP=nAP: v.tensor_copy(nAP, O[:, 0:P]), [O], [numer])
                                    op("dve", lambda v, L=L, dAP=dAP: v.tensor_copy(dAP, L[:, 0:P]), [L], [den])
                                else:
                                    op("dve", lambda v, O=O, nAP=nAP: v.tensor_tensor(nAP, O[:, 0:P], nAP, ALU.add), [O, numer], [numer])
                                    op("dve", lambda v, L=L, dAP=dAP: v.tensor_tensor(dAP, L[:, 0:P], dAP, ALU.add), [L, den], [den])
                first_branch = False
            op("dve", lambda v: v.reciprocal(den[:, :], den[:, :]), [den], [den])
            op("dve", lambda v: v.tensor_tensor(attb[:, :], numer[:, :], den[:, :], ALU.mult), [numer, den], [attb])
            for mo in range(4):
                dma("sp", lambda q, h=h, mo=mo: q.dma_start(out=mixT_scr[mo, :, h, :], in_=attb[:, mo * 512:(mo + 1) * 512]), [attb], [mixT_scr])
        sc.close()
        if stage == 2 and dbg and g == 0:
            cx.wait_all("sp", mixT_scr)
            return nc

    scR.close()
    if True:
        sc = Scope(cx)
        wu = sc.sb("wu", [P, KC, 512], BF16)
        wv = sc.sb("wv", [P, KC, 512], BF16)
        wsT = sc.sb("wsT_s", [P, 4, P], F32)
        wsTb = sc.sb("wsTb", [P, 4, P], BF16)
        bsp = sc.sb("bsp", [P, 4], F32)
        glg = sc.sb("glg", [P, 512], F32)
        glb = sc.sb("glb", [P, 512], F32)
        hTm = [sc.sb("hTu%d" % i, [P, KC, 512], BF16) for i in range(2)]
        ug = [sc.sb("ug%d" % i, [P, 512], F32) for i in range(2)]
        vg = [sc.sb("vg%d" % i, [P, 512], F32) for i in range(2)]
        vnb = [sc.sb("vnb%d" % i, [P, 512], BF16) for i in range(2)]
        gm = [sc.sb("gm%d" % i, [P, 512], BF16) for i in range(2)]
        gmT = sc.sb("gmT", [P, 4, NOWN], BF16)
        st = sc.sb("stg", [P, 4, 6], F32)
        mv = sc.sb("mvg", [P, 4], F32)
        for kc in range(KC):
            dma("pool", lambda q, kc=kc: q.dma_start(out=wu[:, kc, :], in_=w_in[kc * P:(kc + 1) * P, 4608:5120]), [w_in], [wu])
            dma("pool", lambda q, kc=kc: q.dma_start(out=wv[:, kc, :], in_=w_in[kc * P:(kc + 1) * P, 5120:5632]), [w_in], [wv])
        dma("sp", lambda q: q.dma_start(out=wsT[:, :, :], in_=wsT_d.t.rearrange("g s t -> s g t")), [wsT_d], [wsT])
        dma("sp", lambda q: q.dma_start(out=bsp[:, :], in_=bspT_d[:, :]), [bspT_d], [bsp])
        dma("sp", lambda q: q.dma_start(out=glg[:, :], in_=gln_g_d.t.partition_broadcast(P)), [gln_g_d], [glg])
        dma("sp", lambda q: q.dma_start(out=glb[:, :], in_=gln_b_d.t.partition_broadcast(P)), [gln_b_d], [glb])
        for gg in range(4):
            op("dve", lambda v, gg=gg: v.tensor_tensor(wsTb[:, gg, :], wsT[:, gg, :], cst[:, C_TRIU:C_TRIU + P], ALU.mult), [wsT, cst], [wsTb])
        ti = 0
        for m in range(4, 8):
            hT_ = hTm[m % 2]
            dma("sp", lambda q, hT_=hT_, m=m: q.dma_start(out=hT_[:, :, :], in_=hT_scr[m, :, :, :]), [hT_scr], [hT_])
            for sub in range(4):
                ot = (m - 4) * 4 + sub
                u_, v_, vn_, gm_ = ug[ti % 2], vg[ti % 2], vnb[ti % 2], gm[ti % 2]
                ti += 1
                pu, pv = nps(), nps()
                peg([lambda pe, pu=pu, kc=kc, hT_=hT_, sub=sub: pe.matmul(pu[:, :], hT_[:, kc, sub * P:(sub + 1) * P], wu[:, kc, :],
                                                                        start=(kc == 0), stop=(kc == KC - 1)) for kc in range(KC)], [hT_, wu], [pu])
                peg([lambda pe, pv=pv, kc=kc, hT_=hT_, sub=sub: pe.matmul(pv[:, :], hT_[:, kc, sub * P:(sub + 1) * P], wv[:, kc, :],
                                                                        start=(kc == 0), stop=(kc == KC - 1)) for kc in range(KC)], [hT_, wv], [pv])
                op("act", lambda a, pu=pu, u_=u_: a.activation(out=u_[:, :], in_=pu[:, :], func=AF.Gelu), [pu], [u_])
                op("act", lambda a, pv=pv, v_=v_: a.activation(out=v_[:, :], in_=pv[:, :], func=AF.Gelu), [pv], [v_])
                ln_stats(v_, st, mv, width=512)
                op("act", lambda a, v_=v_: a.activation(out=v_[:, :], in_=v_[:, :], func=AF.Identity, bias=mv[:, 3:4], scale=mv[:, 2:3]),
                   [v_, mv], [v_])
                op("dve", lambda v, v_=v_: v.tensor_tensor(v_[:, :], v_[:, :], glg[:, :], ALU.mult), [v_, glg], [v_])
                op("dve", lambda v, v_=v_, vn_=vn_: v.tensor_tensor(vn_[:, :], v_[:, :], glb[:, :], ALU.add), [v_, glb], [vn_])
                pq = nps()
                peg([lambda pe, pq=pq, gg=gg, vn_=vn_: pe.matmul(pq[:, gg * P:(gg + 1) * P], wsTb[:, gg, :], vn_[:, gg * P:(gg + 1) * P],
                                                               start=True, stop=True) for gg in range(4)], [wsTb, vn_], [pq])
                for gg in range(4):
                    op("dve", lambda v, pq=pq, gg=gg, u_=u_, gm_=gm_: v.scalar_tensor_tensor(
                        gm_[:, gg * P:(gg + 1) * P], pq[:, gg * P:(gg + 1) * P], bsp[:, gg:gg + 1], u_[:, gg * P:(gg + 1) * P], ALU.add, ALU.mult),
                       [pq, bsp, u_], [gm_])
                pt_ = nps()
                peg([lambda pe, pt_=pt_, gg=gg, gm_=gm_: pe.transpose(bfview(pt_)[:, gg * P:(gg + 1) * P], gm_[:, gg * P:(gg + 1) * P], identb[:, :])
                     for gg in range(4)], [gm_, identb], [pt_])
                op("act", lambda a, pt_=pt_, ot=ot: a.activation(out=gmT[:, :, ot * P:(ot + 1) * P],
                                                                in_=bfview(pt_, 512).rearrange("p (k t) -> p k t", t=P), func=AF.Identity),
                   [pt_], [gmT])
        for mo in range(4):
            dma("sp", lambda q, mo=mo: q.dma_start(out=mixT_scr[mo, :, 12:16, :], in_=gmT[:, :, mo * 512:(mo + 1) * 512]), [gmT], [mixT_scr])
        sc.close()

    if stage == 2:
        cx.wait_all("sp", mixT_scr)
        return nc

    w_out_d = din("w_out", [D, D])
    ln1_g_d = din("ln1_g", [D])
    ln1_b_d = din("ln1_b", [D])
    router_w_d = din("router_w", [D, NE])
    router_b_d = din("router_b", [NE])
    x1_scr = dscr("x1_scr", [NOWN, D], F32)
    Xs = dscr("Xs", [NSLOT, D], BF16)
    if True:
        sc = Scope(cx)
        wob = sc.sb("wob", [P, KC, D], BF16)
        g1bc = sc.sb("g1bc", [P, D], F32)
        l1g = sc.sb("l1g", [P, D], F32)
        l1b = sc.sb("l1b", [P, D], F32)
        sc2p = sc.sb("sc2p", [P, D], F32)
        sh2b = sc.sb("sh2b", [P, D], F32)
        mixTm = [sc.sb("mixTm%d" % i, [P, KC, 512], BF16) for i in range(2)]
        xt = [sc.sb("xt3_%d" % i, [P, D], F32) for i in range(2)]
        y1 = sc.sb("y1", [P, D], F32)
        x1t = sc.sb("x1t", [P, D], F32)
        h2b = sc.sb("h2b", [P, D], BF16)
        h2T = sc.sb("h2T", [P, KC, P], F32)
        rw = sc.sb("rw", [P, KC, NE], F32)
        rbb = sc.sb("rbb", [P, NE], F32)
        lg = sc.sb("lg", [P, NE], F32)
        m8 = sc.sb("m8", [P, 8], F32)
        sm = sc.sb("sm", [P, 8], F32)
        mkf = sc.sb("mkf", [P, NE], F32)
        mkb = sc.sb("mkb", [P, NE], BF16)
        ustrb = sc.sb("ustrb", [P, P], BF16)
        cntp = sc.sb("cntp", [P, NE], F32)
        slotf = sc.sb("slotf", [P, NE], F32)
        prod = sc.sb("prod", [P, NE], F32)
        slot4 = sc.sb("slot4", [P, 4], F32)
        st = sc.sb("st3", [P, 4, 6], F32)
        mv = sc.sb("mv3", [P, 4], F32)
        for kc in range(KC):
            dma("pool", lambda q, kc=kc: q.dma_start(out=wob[:, kc, :], in_=w_out_d[kc * P:(kc + 1) * P, :]), [w_out_d], [wob])
        dma("sp", lambda q: q.dma_start(out=g1bc[:, :], in_=mod_scr.t[2, :].partition_broadcast(P)), [mod_scr], [g1bc])
        dma("sp", lambda q: q.dma_start(out=sh2b[:, :], in_=mod_scr.t[3, :].partition_broadcast(P)), [mod_scr], [sh2b])
        dma("sp", lambda q: q.dma_start(out=sc2p[:, :], in_=mod_scr.t[4, :].partition_broadcast(P)), [mod_scr], [sc2p])
        dma("sp", lambda q: q.dma_start(out=l1g[:, :], in_=ln1_g_d.t.partition_broadcast(P)), [ln1_g_d], [l1g])
        dma("sp", lambda q: q.dma_start(out=l1b[:, :], in_=ln1_b_d.t.partition_broadcast(P)), [ln1_b_d], [l1b])
        dma("sp", lambda q: q.dma_start(out=rw[:, :, :], in_=router_w_d.t.rearrange("(kc p) e -> p kc e", p=P)), [router_w_d], [rw])
        dma("sp", lambda q: q.dma_start(out=rbb[:, :], in_=router_b_d.t.partition_broadcast(P)), [router_b_d], [rbb])
        op("dve", lambda v: v.tensor_scalar_add(sc2p[:, :], sc2p[:, :], 1.0), [sc2p], [sc2p])
        op("dve", lambda v: v.tensor_copy(ustrb[:, :], cst[:, C_USTR:C_USTR + P]), [cst], [ustrb])
        op("dve", lambda v: v.memset(cntp[:, :], 0.0), [], [cntp])
        for ot in range(16):
            m, sub = ot // 4, ot % 4
            mt_ = mixTm[m % 2]
            x_ = xt[ot % 2]
            if sub == 0:
                dma("sp", lambda q, mt_=mt_, m=m: q.dma_start(out=mt_[:, :, :], in_=mixT_scr[m, :, :, :]), [mixT_scr], [mt_])
            dma("sp", lambda q, x_=x_, ot=ot: q.dma_start(out=x_[:, :], in_=xin[NOWN + ot * P:NOWN + (ot + 1) * P, :]), [xin], [x_])
            for n in range(4):
                pb = nps()
                peg([lambda pe, pb=pb, c=c, n=n, mt_=mt_, sub=sub: pe.matmul(pb[:, :], mt_[:, c, sub * P:(sub + 1) * P], wob[:, c, n * 512:(n + 1) * 512],
                                                                          start=(c == 0), stop=(c == KC - 1)) for c in range(KC)], [mt_, wob], [pb])
                op("dve", lambda v, pb=pb, n=n: v.tensor_tensor(y1[:, n * 512:(n + 1) * 512], pb[:, :], g1bc[:, n * 512:(n + 1) * 512], ALU.mult),
                   [pb, g1bc], [y1])
            op("dve", lambda gp, x_=x_: gp.scalar_tensor_tensor(y1[:, :], x_[:, :], ALPHA, y1[:, :], ALU.mult, ALU.add), [x_, y1], [y1])
            ln_stats(y1, st, mv)
            op("act", lambda a: a.activation(out=x1t[:, :], in_=y1[:, :], func=AF.Identity, bias=mv[:, 3:4], scale=mv[:, 2:3]), [y1, mv], [x1t])
            op("dve", lambda v: v.tensor_tensor(x1t[:, :], x1t[:, :], l1g[:, :], ALU.mult), [x1t, l1g], [x1t])
            op("pool", lambda gp: gp.tensor_tensor(x1t[:, :], x1t[:, :], l1b[:, :], ALU.add), [x1t, l1b], [x1t])
            dma("sp", lambda q, ot=ot: q.dma_start(out=x1_scr[ot * P:(ot + 1) * P, :], in_=x1t[:, :]), [x1t], [x1_scr])
            ln_stats(x1t, st, mv)
            op("act", lambda a: a.activation(out=y1[:, :], in_=x1t[:, :], func=AF.Identity, bias=mv[:, 3:4], scale=mv[:, 2:3]), [x1t, mv], [y1])
            op("dve", lambda v: v.tensor_tensor(y1[:, :], y1[:, :], sc2p[:, :], ALU.mult), [y1, sc2p], [y1])
            op("pool", lambda gp: gp.tensor_tensor(y1[:, :], y1[:, :], sh2b[:, :], ALU.add), [y1, sh2b], [y1])
            op("act", lambda a: a.activation(out=h2b[:, :], in_=y1[:, :], func=AF.Identity), [y1], [h2b])
            for qd in range(4):
                pb = nps()
                peg([lambda pe, pb=pb, k=k, qd=qd: pe.transpose(pb[:, k * P:(k + 1) * P], y1[:, (qd * 4 + k) * P:(qd * 4 + k + 1) * P], cst[:, C_ID:C_ID + P])
                     for k in range(4)], [y1, cst], [pb])
                op("act", lambda a, pb=pb, qd=qd: a.activation(out=h2T[:, qd * 4:(qd + 1) * 4, :], in_=pb[:, :].rearrange("p (k t) -> p k t", t=P),
                                                            func=AF.Identity), [pb], [h2T])
            pl = nps()
            peg([lambda pe, pl=pl, kc=kc: pe.matmul(pl[:, 0:NE], h2T[:, kc, :], rw[:, kc, :], start=(kc == 0), stop=(kc == KC - 1)) for kc in range(KC)],
                [h2T, rw], [pl])
            op("dve", lambda v, pl=pl: v.tensor_tensor(lg[:, :], pl[:, 0:NE], rbb[:, :], ALU.add), [pl, rbb], [lg])
            op("dve", lambda v: v.max(m8[:, :], lg[:, :]), [lg], [m8])
            op("dve", lambda v: v.tensor_scalar(sm[:, 0:1], m8[:, 0:1], -1.0, None, ALU.mult), [m8], [sm])
            op("act", lambda a: a.activation(out=sm[:, 4:8], in_=m8[:, 0:4], func=AF.Exp, bias=sm[:, 0:1], scale=1.0), [m8, sm], [sm])
            op("dve", lambda v: v.tensor_reduce(sm[:, 1:2], sm[:, 4:8], AX.X, ALU.add), [sm], [sm])
            op("dve", lambda v: v.reciprocal(sm[:, 2:3], sm[:, 1:2]), [sm], [sm])
            op("dve", lambda v, ot=ot: v.tensor_scalar(gates[:, ot, :], sm[:, 4:8], sm[:, 2:3], None, ALU.mult), [sm], [gates])
            op("dve", lambda v: v.tensor_scalar(mkf[:, :], lg[:, :], m8[:, 3:4], None, ALU.is_ge), [lg, m8], [mkf])
            op("dve", lambda v: v.tensor_copy(mkb[:, :], mkf[:, :]), [mkf], [mkb])
            pr = nps()
            peg([lambda pe, pr=pr: pe.matmul(pr[:, 0:NE], ustrb[:, :], mkb[:, :], start=True, stop=True),
                 lambda pe, pr=pr: pe.matmul(pr[:, NE:2 * NE], onesb[:, :], mkb[:, :], start=True, stop=True)], [ustrb, onesb, mkb], [pr])
            op("dve", lambda v, pr=pr: v.tensor_tensor(slotf[:, :], pr[:, 0:NE], cntp[:, :], ALU.add), [pr, cntp], [slotf])
            op("dve", lambda v: v.tensor_scalar(prod[:, :], slotf[:, :], float(CAP), 1.0e6, ALU.is_ge, ALU.mult), [slotf], [prod])
            op("dve", lambda v: v.tensor_tensor(slotf[:, :], slotf[:, :], cst[:, C_EOFF:C_EOFF + NE], ALU.add), [slotf, cst], [slotf])
            op("dve", lambda v: v.tensor_tensor(slotf[:, :], slotf[:, :], prod[:, :], ALU.add), [slotf, prod], [slotf])
            op("dve", lambda v, pr=pr: v.tensor_tensor(cntp[:, :], pr[:, NE:2 * NE], cntp[:, :], ALU.add), [pr, cntp], [cntp])
            for k in range(4):
                op("dve", lambda v, k=k: v.scalar_tensor_tensor(prod[:, :], lg[:, :], m8[:, k:k + 1], slotf[:, :], ALU.is_equal, ALU.mult),
                   [lg, m8, slotf], [prod])
                op("dve", lambda v, k=k: v.tensor_reduce(slot4[:, k:k + 1], prod[:, :], AX.X, ALU.add), [prod], [slot4])
            op("dve", lambda v, ot=ot: v.tensor_copy(slot_i[:, ot * 4:ot * 4 + 4], slot4[:, :]), [slot4], [slot_i])
            op("dve", lambda v: v.tensor_scalar(sm[:, 4:8], slot4[:, :], float(NSLOT), None, ALU.is_lt), [slot4], [sm])
            op("dve", lambda v, ot=ot: v.tensor_tensor(gates[:, ot, :], gates[:, ot, :], sm[:, 4:8], ALU.mult), [gates, sm], [gates])
            ix = idxs[ot % 2]
            op("dve", lambda v, ix=ix: v.tensor_copy(ix[:, :], slot4[:, :]), [slot4], [ix])
            for k in range(4):
                dma("pool", lambda q, ix=ix, k=k: q.indirect_dma_start(
                    out=Xs[:, :], out_offset=bass.IndirectOffsetOnAxis(ap=ix[:, k:k + 1], axis=0),
                    in_=h2b[:, :], in_offset=None, bounds_check=bchk, oob_is_err=False), [h2b, ix], [Xs])
        sc.close()

    if stage == 3:
        dbg3 = dscr("dbg3", [P, 16, 8], F32, out=True)
        if True:
            sc = Scope(cx)
            tmp = sc.sb("dbgtmp", [P, 16, 8], F32)
            op("dve", lambda v: v.tensor_copy(tmp[:, :, 0:4], slot_i[:, :].rearrange("p (t k) -> p t k", k=4)), [slot_i], [tmp])
            op("dve", lambda v: v.tensor_copy(tmp[:, :, 4:8], gates[:, :, :]), [gates], [tmp])
            dma("sp", lambda q: q.dma_start(out=dbg3[:, :, :], in_=tmp[:, :, :]), [tmp], [dbg3])
            cx.wait_all("sp", dbg3)
            cx.wait_all("sp", x1_scr)
            cx.wait_all("sp", Xs)
            sc.close()
        return nc

    w_gu_d = din("w_gu", [NE, D, 2 * D])
    w_d_d = din("w_d", [NE, D, D])
    bguT_d = din("bguT", [NE, P, 32])
    b_d_d = din("b_d", [NE, D])
    ln2_g_d = din("ln2_g", [D])
    ln2_b_d = din("ln2_b", [D])
    Y = dscr("Y", [NSLOT, D], F32)
    NSB = CAP // P
    NHALF = CAP // 512
    if True:
        sc = Scope(cx)
        stg = [sc.sb("stg%d" % i, [P, 8, 512], F32) for i in range(2)]
        wbf = [sc.sb("wbf%d" % i, [P, KC, 512], BF16) for i in range(3)]
        xrow = [sc.sb("xrow%d" % i, [P, D], BF16) for i in range(2)]
        xT = sc.sb("xT", [P, KC, CAP], BF16)
        actT = sc.sb("actT", [P, KC, CAP], BF16)
        yst = [sc.sb("yst%d" % i, [P, 512], F32) for i in range(3)]
        bdbc = [sc.sb("bdbc%d" % i, [P, D], BF16) for i in range(2)]
        bgu = [sc.sb("bgu%d" % i, [P, 32], F32) for i in range(2)]
        gt = [sc.sb("gt%d" % i, [P, 512], F32) for i in range(2)]
        ut = [sc.sb("ut%d" % i, [P, 512], F32) for i in range(2)]
        sg = [sc.sb("sg%d" % i, [P, 512], F32) for i in range(2)]
        NW = len(wbf)

        gran = []
        for e in range(NE):
            for q4 in range(4):
                gran.append((e, 0, q4))
                gran.append((e, 1, q4))
            for n in range(4):
                gran.append((e, 2, n))
        emitted = [0]
        cast_rot = ["dve", "act", "dve", "pool"]
        hcount = [0]

        def emit_load(i):
            e, kind, idx = gran[i]
            wb = wbf[i % NW]
            for half in range(2):
                s_ = stg[hcount[0] % len(stg)]
                eng = cast_rot[hcount[0] % 4]
                hcount[0] += 1
                if kind == 2:
                    src = w_d_d.t[e].rearrange("(kc p) n -> p kc n", p=P)[:, half * 8:(half + 1) * 8, idx * 512:(idx + 1) * 512]
                    dep = w_d_d
                else:
                    c0 = kind * D + idx * 512
                    src = w_gu_d.t[e].rearrange("(kc p) n -> p kc n", p=P)[:, half * 8:(half + 1) * 8, c0:c0 + 512]
                    dep = w_gu_d
                dma("sp", lambda q, s_=s_, src=src: q.dma_start(out=s_[:, :, :], in_=src), [dep], [s_])
                if eng == "act":
                    op("act", lambda a, wb=wb, half=half, s_=s_: a.activation(out=wb[:, half * 8:(half + 1) * 8, :], in_=s_[:, :, :], func=AF.Identity),
                       [s_], [wb])
                else:
                    op(eng, lambda v, wb=wb, half=half, s_=s_: v.tensor_copy(wb[:, half * 8:(half + 1) * 8, :], s_[:, :, :]), [s_], [wb])

        def ensure(upto):
            while emitted[0] < min(upto, len(gran)):
                emit_load(emitted[0])
                emitted[0] += 1

        gi = 0
        ensure(2)
        cnt2 = 0
        ycnt = 0
        for e in range(NE):
            bg_, bd_ = bgu[e % 2], bdbc[e % 2]
            dma("sp", lambda q, e=e, bg_=bg_: q.dma_start(out=bg_[:, :], in_=bguT_d[e, :, :]), [bguT_d], [bg_])
            dma("pool", lambda q, e=e, bd_=bd_: q.dma_start(out=bd_[:, :], in_=b_d_d.t[e, :].partition_broadcast(P)), [b_d_d], [bd_])
            for sbk in range(NSB):
                xr = xrow[(e * NSB + sbk) % 2]
                r0 = e * CAP + sbk * P
                dma("sp", lambda q, xr=xr, r0=r0: q.dma_start(out=xr[:, :], in_=Xs[r0:r0 + P, :]), [Xs], [xr])
                for half in range(2):
                    pb = nps()
                    peg([lambda pe, pb=pb, k=k, half=half, xr=xr: pe.transpose(bfview(pb)[:, k * P:(k + 1) * P],
                                                                             xr[:, (half * 8 + k) * P:(half * 8 + k + 1) * P], identb[:, :])
                         for k in range(8)], [xr, identb], [pb])
                    op("act", lambda a, pb=pb, half=half, sbk=sbk: a.activation(
                        out=xT[:, half * 8:(half + 1) * 8, sbk * P:(sbk + 1) * P],
                        in_=bfview(pb).rearrange("p (k t) -> p k t", t=P), func=AF.Identity), [pb], [xT])
            for q4 in range(4):
                ensure(gi + 3)
                wg_, wu_ = wbf[gi % NW], wbf[(gi + 1) % NW]
                for jj in range(4):
                    j = q4 * 4 + jj
                    for nh in range(NHALF):
                        g_, u_, s_g = gt[cnt2 % 2], ut[cnt2 % 2], sg[cnt2 % 2]
                        cnt2 += 1
                        pg, pu = nps(), nps()
                        c0 = nh * 512
                        peg([lambda pe, pg=pg, kc=kc, jj=jj, wg_=wg_, c0=c0: pe.matmul(pg[:, :], wg_[:, kc, jj * P:(jj + 1) * P], xT[:, kc, c0:c0 + 512],
                                                                                      start=(kc == 0), stop=(kc == KC - 1)) for kc in range(KC)], [wg_, xT], [pg])
                        peg([lambda pe, pu=pu, kc=kc, jj=jj, wu_=wu_, c0=c0: pe.matmul(pu[:, :], wu_[:, kc, jj * P:(jj + 1) * P], xT[:, kc, c0:c0 + 512],
                                                                                      start=(kc == 0), stop=(kc == KC - 1)) for kc in range(KC)], [wu_, xT], [pu])
                        op("dve", lambda v, pg=pg, g_=g_, j=j, bg_=bg_: v.tensor_scalar(g_[:, :], pg[:, :], bg_[:, j:j + 1], 7.0, ALU.add, ALU.min),
                           [pg, bg_], [g_])
                        op("dve", lambda v, pu=pu, u_=u_, j=j, bg_=bg_: v.tensor_scalar(u_[:, :], pu[:, :], bg_[:, 16 + j:17 + j], 7.0, ALU.add, ALU.min),
                           [pu, bg_], [u_])
                        op("pool", lambda gp, u_=u_: gp.tensor_scalar(u_[:, :], u_[:, :], -7.0, 1.0, ALU.max, ALU.add), [u_], [u_])
                        op("act", lambda a, g_=g_, s_g=s_g: a.activation(out=s_g[:, :], in_=g_[:, :], func=AF.Sigmoid, scale=1.702), [g_], [s_g])
                        op("pool", lambda gp, g_=g_, s_g=s_g: gp.tensor_tensor(g_[:, :], g_[:, :], s_g[:, :], ALU.mult), [g_, s_g], [g_])
                        op("dve", lambda v, g_=g_, u_=u_, j=j, c0=c0: v.tensor_tensor(actT[:, j, c0:c0 + 512], g_[:, :], u_[:, :], ALU.mult), [g_, u_], [actT])
                gi += 2
            for n in range(4):
                ensure(gi + 2)
                wd_ = wbf[gi % NW]
                for sbk in range(NSB):
                    ys = yst[ycnt % 3]
                    ycnt += 1
                    pb = nps()
                    peg([lambda pe, pb=pb, j=j, sbk=sbk, wd_=wd_: pe.matmul(pb[:, :], actT[:, j, sbk * P:(sbk + 1) * P], wd_[:, j, :],
                                                                           start=(j == 0), stop=(j == KC - 1)) for j in range(KC)], [actT, wd_], [pb])
                    op("dve", lambda v, pb=pb, ys=ys, n=n, bd_=bd_: v.tensor_tensor(ys[:, :], pb[:, :], bd_[:, n * 512:(n + 1) * 512], ALU.add),
                       [pb, bd_], [ys])
                    r0 = e * CAP + sbk * P
                    dma("sp", lambda q, ys=ys, r0=r0, n=n: q.dma_start(out=Y[r0:r0 + P, n * 512:(n + 1) * 512], in_=ys[:, :]), [ys], [Y])
                gi += 1
        sc.close()

    if True:
        sc = Scope(cx)
        g2bc = sc.sb("g2bc", [P, D], F32)
        l2g = sc.sb("l2g", [P, D], F32)
        l2b = sc.sb("l2b", [P, D], F32)
        yk = [sc.sb("yk%d" % i, [P, D], F32) for i in range(4)]
        x1r = [sc.sb("x1r%d" % i, [P, D], F32) for i in range(2)]
        acc = sc.sb("acc", [P, D], F32)
        ot_ = [sc.sb("outt%d" % i, [P, D], F32) for i in range(2)]
        st = sc.sb("st4", [P, 4, 6], F32)
        mv = sc.sb("mv4", [P, 4], F32)
        for k in range(4):
            op("dve", lambda v, k=k: v.memset(yk[k][:, :], 0.0), [], [yk[k]])
        dma("sp", lambda q: q.dma_start(out=g2bc[:, :], in_=mod_scr.t[5, :].partition_broadcast(P)), [mod_scr], [g2bc])
        dma("sp", lambda q: q.dma_start(out=l2g[:, :], in_=ln2_g_d.t.partition_broadcast(P)), [ln2_g_d], [l2g])
        dma("sp", lambda q: q.dma_start(out=l2b[:, :], in_=ln2_b_d.t.partition_broadcast(P)), [ln2_b_d], [l2b])
        for ot in range(16):
            x1_ = x1r[ot % 2]
            o_ = ot_[ot % 2]
            dma("sp", lambda q, x1_=x1_, ot=ot: q.dma_start(out=x1_[:, :], in_=x1_scr[ot * P:(ot + 1) * P, :]), [x1_scr], [x1_])
            ix = idxs[ot % 2]
            op("dve", lambda v, ix=ix, ot=ot: v.tensor_copy(ix[:, :], slot_i[:, ot * 4:ot * 4 + 4]), [slot_i], [ix])
            for k in range(4):
                dma("pool", lambda q, ix=ix, k=k: q.indirect_dma_start(
                    out=yk[k][:, :], out_offset=None, in_=Y[:, :],
                    in_offset=bass.IndirectOffsetOnAxis(ap=ix[:, k:k + 1], axis=0), bounds_check=bchk, oob_is_err=False),
                    [Y, ix], [yk[k]])
            op("dve", lambda v, ot=ot: v.tensor_scalar(acc[:, :], yk[0][:, :], gates[:, ot, 0:1], None, ALU.mult), [yk[0], gates], [acc])
            for k in range(1, 4):
                op("dve", lambda v, ot=ot, k=k: v.scalar_tensor_tensor(acc[:, :], yk[k][:, :], gates[:, ot, k:k + 1], acc[:, :], ALU.mult, ALU.add),
                   [yk[k], gates, acc], [acc])
            op("pool", lambda gp: gp.tensor_tensor(acc[:, :], acc[:, :], g2bc[:, :], ALU.mult), [acc, g2bc], [acc])
            op("dve", lambda v, x1_=x1_: v.scalar_tensor_tensor(acc[:, :], x1_[:, :], ALPHA, acc[:, :], ALU.mult, ALU.add), [x1_, acc], [acc])
            ln_stats(acc, st, mv)
            op("act", lambda a, o_=o_: a.activation(out=o_[:, :], in_=acc[:, :], func=AF.Identity, bias=mv[:, 3:4], scale=mv[:, 2:3]), [acc, mv], [o_])
            op("pool", lambda gp, o_=o_: gp.tensor_tensor(o_[:, :], o_[:, :], l2g[:, :], ALU.mult), [o_, l2g], [o_])
            op("dve", lambda v, o_=o_: v.tensor_tensor(o_[:, :], o_[:, :], l2b[:, :], ALU.add), [o_, l2b], [o_])
            dma("sp", lambda q, o_=o_, ot=ot: q.dma_start(out=out_d[ot * P:(ot + 1) * P, :], in_=o_[:, :]), [o_], [out_d])
        cx.wait_all("sp", out_d)
        sc.close()
    return nc


def make_in_maps(inputs, stage=99):
    x = np.asarray(inputs["x"], np.float32)
    c = np.asarray(inputs["c"], np.float32)
    pos = np.asarray(inputs["positions"], np.int32)
    consts = make_consts()
    maps = []
    for core in range(8):
        b, half = core // 2, core % 2
        m = {}
        if half == 0:
            xin = np.concatenate([np.zeros((NOWN, D), np.float32), x[b, 0:NOWN]], axis=0)
            pp = np.concatenate([np.zeros((NOWN,), np.int32), pos[b, 0:NOWN]])
            hm = np.full((P, 1), -BIG, np.float32)
        else:
            xin = x[b]
            pp = pos[b]
            hm = np.zeros((P, 1), np.float32)
        m["xin"] = np.ascontiguousarray(xin)
        m["posb"] = np.ascontiguousarray(pp)
        m["cvec"] = np.ascontiguousarray(c[b].reshape(KC, P).T)
        m["hmask"] = hm
        m["consts"] = consts
        m["ada_w"] = np.asarray(inputs["ada_w"], np.float32)[0]
        m["ada_b"] = np.asarray(inputs["ada_b"], np.float32)[0]
        m["w_in"] = np.asarray(inputs["w_in"], np.float32)[0]
        if stage >= 2:
            m["wsT"] = np.ascontiguousarray(np.asarray(inputs["w_spatial"], np.float32)[0].transpose(0, 2, 1))
            m["bspT"] = np.ascontiguousarray(np.asarray(inputs["b_spatial"], np.float32)[0].T)
            m["gln_g"] = np.asarray(inputs["gmlp_ln_g"], np.float32)[0]
            m["gln_b"] = np.asarray(inputs["gmlp_ln_b"], np.float32)[0]
        if stage >= 3:
            m["w_out"] = np.asarray(inputs["w_out"], np.float32)[0]
            m["ln1_g"] = np.asarray(inputs["ln1_g"], np.float32)[0]
            m["ln1_b"] = np.asarray(inputs["ln1_b"], np.float32)[0]
            m["router_w"] = np.asarray(inputs["router_w"], np.float32)[0]
            m["router_b"] = np.asarray(inputs["router_b"], np.float32)[0]
        if stage >= 4:
            m["w_gu"] = np.asarray(inputs["w_gate_up"], np.float32)[0]
            m["w_d"] = np.asarray(inputs["w_down"], np.float32)[0]
            bgu = np.asarray(inputs["b_gate_up"], np.float32)[0]
            m["bguT"] = np.ascontiguousarray(bgu.reshape(NE, 32, P).transpose(0, 2, 1))
            m["b_d"] = np.asarray(inputs["b_down"], np.float32)[0]
            m["ln2_g"] = np.asarray(inputs["ln2_g"], np.float32)[0]
            m["ln2_b"] = np.asarray(inputs["ln2_b"], np.float32)[0]
        maps.append(m)
    return maps


def kernel(**inputs):
    nc = build()
    maps = make_in_maps(inputs)
    res = run_bass_kernel_spmd(nc, maps, core_ids=list(range(8)))
    out = np.zeros((4, 4096, D), np.float32)
    for core in range(8):
        b, half = core // 2, core % 2
        out[b, half * NOWN:(half + 1) * NOWN] = res.results[core]["out"]
    return out
```

```python
import os
import numpy as np
from contextlib import ExitStack
import concourse.bass as bass
import concourse.mybir as mybir
from concourse.bass_utils import run_bass_kernel_spmd

F32 = mybir.dt.float32
BF16 = mybir.dt.bfloat16
I32 = mybir.dt.int32
AF = mybir.ActivationFunctionType
ALU = mybir.AluOpType
AX = mybir.AxisListType

P = 128
D = 2048
KC = 16
NOWN = 2048
NALL = 4096
NH = 12
HG = 3
NG = NH // HG
NE = 32
CAP = 1024
NSLOT = NE * CAP
ALPHA = float(2.0 ** 0.25)
EPS = 1e-5
BIG = 30000.0
SM_SCALE = float(1.0 / np.sqrt(128.0))
TWO_PI = float(2.0 * np.pi)

C_ID, C_MASK, C_SWAP, C_TRIU, C_USTR, C_EOFF, C_INVF, C_SGN, C_END = 0, 128, 384, 512, 640, 768, 800, 801, 802


def make_consts():
    c = np.zeros((128, C_END), np.float32)
    i = np.arange(128)
    c[:, C_ID:C_ID + 128] = np.eye(128, dtype=np.float32)
    k = i[:, None]
    q = i[None, :]
    c[:, C_MASK:C_MASK + 128] = np.where(k <= q, 0.0, -BIG)
    c[:, C_MASK + 128:C_MASK + 256] = np.where(k >= q, 0.0, -BIG)
    sw = np.zeros((128, 128), np.float32)
    sw[(i + 64) % 128, i] = 1.0
    c[:, C_SWAP:C_SWAP + 128] = sw
    c[:, C_TRIU:C_TRIU + 128] = (k <= q).astype(np.float32)
    c[:, C_USTR:C_USTR + 128] = (k < q).astype(np.float32)
    c[:, C_EOFF:C_EOFF + 32] = (np.arange(32) * CAP)[None, :].astype(np.float32)
    invf = np.power(np.float32(10000.0), -np.arange(64, dtype=np.float32) * np.float32(2.0 / 128)).astype(np.float32)
    c[:, C_INVF] = invf[i % 64]
    c[:, C_SGN] = np.where(i < 64, -1.0, 1.0)
    return c


def sl(start, n, step):
    return slice(start, start + (n - 1) * step + 1, step)


class Buf:
    def __init__(self, t):
        self.t = t
        self.w = {}
        self.r = {}
        self.excl = False

    def __getitem__(self, k):
        return self.t[k]


class Ctx:
    def __init__(self, nc):
        self.nc = nc
        self.E = dict(pe=nc.tensor, act=nc.scalar, dve=nc.vector, pool=nc.gpsimd, sp=nc.sync)
        self.csem = {}
        self.ccnt = {}
        for e in ("pe", "act", "dve", "pool"):
            self.csem[e] = nc.alloc_semaphore(name="c_" + e)
            self.ccnt[e] = 0
        self.waited = {e: {} for e in self.E}
        self.dsems = {}
        self.dcnt = {}
        self.dnext = {}
        for q, n in (("sp", 12), ("pool", 10), ("act", 4)):
            self.dsems[q] = [nc.alloc_semaphore(name="d_%s%d" % (q, i)) for i in range(n)]
            self.dnext[q] = 0
        self.semname = {}
        self.rec = None
        self.regs = {}

    def _key(self, sem):
        return id(sem)

    def wait_all(self, e, b):
        for t in list(b.w.values()):
            self.wait(e, t)

    def wait(self, e, tok):
        if tok is None:
            return
        sem, val = tok
        if e == "pe" and sem is self.csem["pe"]:
            return
        k = self._key(sem)
        if self.waited[e].get(k, 0) >= val:
            return
        if self.rec is not None:
            self.rec[e].append(lambda E=self.E[e], sem=sem, val=val: E.wait_ge(sem, val))
        else:
            self.E[e].wait_ge(sem, val)
        self.waited[e][k] = val

    @staticmethod
    def _split(reads, writes):
        writes = list(writes) + [b for b in reads if b.excl and b not in writes]
        reads = [b for b in reads if not b.excl]
        return reads, writes

    def _deps(self, e, reads, writes):
        for b in reads:
            for t in list(b.w.values()):
                self.wait(e, t)
        for b in writes:
            for t in list(b.w.values()):
                self.wait(e, t)
            for t in list(b.r.values()):
                self.wait(e, t)

    def _commit(self, tok, reads, writes):
        for b in reads:
            k = self._key(tok[0])
            old = b.r.get(k)
            if old is None or old[1] < tok[1]:
                b.r[k] = tok
        for b in writes:
            k = self._key(tok[0])
            if tok[0] in self.csem.values():
                b.w = {k: tok}
            else:
                b.w[k] = tok
            b.r = {}

    def release(self, bufs):
        for e in ("pe", "act", "dve", "pool", "sp"):
            for b in bufs:
                for t in list(b.w.values()):
                    self.wait(e, t)
                for t in list(b.r.values()):
                    self.wait(e, t)

    def op(self, e, fn, reads=(), writes=()):
        reads, writes = self._split(reads, writes)
        self._deps(e, reads, writes)
        self.ccnt[e] += 1
        if self.rec is not None:
            self.rec[e].append(lambda E=self.E[e], fn=fn, sem=self.csem[e]: fn(E).then_inc(sem, 1))
        else:
            fn(self.E[e]).then_inc(self.csem[e], 1)
        tok = (self.csem[e], self.ccnt[e])
        self._commit(tok, reads, writes)
        return tok

    def region(self, engines, thr):
        return _Region(self, engines, thr)

    def pe_group(self, fns, reads=(), writes=()):
        reads, writes = self._split(reads, writes)
        self._deps("pe", reads, writes)
        self.ccnt["pe"] += 1

        def emit(E=self.E["pe"], fns=list(fns), sem=self.csem["pe"]):
            inst = None
            for fn in fns:
                inst = fn(E)
            inst.then_inc(sem, 1)
        if self.rec is not None:
            self.rec["pe"].append(emit)
        else:
            emit()
        tok = (self.csem["pe"], self.ccnt["pe"])
        self._commit(tok, reads, writes)
        return tok

    def dma(self, q, fn, reads=(), writes=()):
        sems = self.dsems[q]
        s = sems[self.dnext[q] % len(sems)]
        self.dnext[q] += 1
        k = self._key(s)
        n = self.dcnt.get(k, 0)
        if n > 0:
            self.wait(q, (s, 16 * n))
        self._deps(q, reads, writes)
        if self.rec is not None:
            self.rec[q].append(lambda E=self.E[q], fn=fn, s=s: fn(E).then_inc(s, 16))
        else:
            fn(self.E[q]).then_inc(s, 16)
        self.dcnt[k] = n + 1
        tok = (s, 16 * (n + 1))
        self._commit(tok, reads, writes)
        return tok


class _Region:
    def __init__(self, cx, engines, thr):
        self.cx, self.engines, self.thr = cx, engines, thr

    def __enter__(self):
        cx = self.cx
        assert cx.rec is None
        self.c0 = dict(cx.ccnt)
        self.d0 = dict(cx.dcnt)
        self.w0 = {e: dict(d) for e, d in cx.waited.items()}
        cx.rec = {e: [] for e in cx.E}
        return self

    def __exit__(self, *a):
        cx = self.cx
        rec = cx.rec
        cx.rec = None
        for e in cx.E:
            if not rec[e]:
                continue
            assert e in self.engines, e
            E = cx.E[e]
            with E.If_cmp(cx.regs[e], self.thr, "IS_GE"):
                for t in rec[e]:
                    t()
            with E.Else():
                if e in cx.csem and cx.ccnt[e] > self.c0[e]:
                    if self.c0[e] > 0:
                        E.wait_ge(cx.csem[e], self.c0[e])
                    E.sem_inc(cx.csem[e], cx.ccnt[e] - self.c0[e])
                if e in cx.dsems:
                    for s_ in cx.dsems[e]:
                        k = id(s_)
                        n0, n1 = self.d0.get(k, 0), cx.dcnt.get(k, 0)
                        if n1 > n0:
                            if n0 > 0:
                                E.wait_ge(s_, 16 * n0)
                            E.sem_inc(s_, 16 * (n1 - n0))
                E.nop()
        cx.waited = self.w0
        return False


class Scope:
    def __init__(self, cx):
        self.cx = cx
        self.nc = cx.nc
        self.bufs = []
        self.stack = ExitStack()

    _n = [0]

    def sb(self, name, shape, dt):
        Scope._n[0] += 1
        name = "%s_%d" % (name, Scope._n[0])
        b = Buf(self.stack.enter_context(self.nc.sbuf_tensor(name, list(shape), dt)))
        self.bufs.append(b)
        return b

    def close(self):
        self.cx.release(self.bufs)
        self.stack.close()


def build(stage=99, dbg=False):
    nc = bass.Bass("TRN2", target_bir_lowering=False)
    cx = Ctx(nc)
    op, dma, peg = cx.op, cx.dma, cx.pe_group

    def din(name, shape, dt=F32):
        return Buf(nc.dram_tensor(name, list(shape), dt, kind="ExternalInput").ap())

    def dscr(name, shape, dt, out=False):
        kind = "ExternalOutput" if (out or dbg) else "Internal"
        return Buf(nc.dram_tensor(name, list(shape), dt, kind=kind).ap())

    def sb(name, shape, dt):
        return Buf(nc.alloc_sbuf_tensor(name, list(shape), dt))

    def psum(name, shape, dt=F32):
        b = Buf(nc.alloc_psum_tensor(name, list(shape), dt))
        b.excl = True
        return b

    xin = din("xin", [NALL, D])
    posb = din("posb", [NALL], I32)
    cvec = din("cvec", [P, KC])
    hmask_d = din("hmask", [P, 1])
    consts_d = din("consts", [P, C_END])
    ada_w = din("ada_w", [D, 6 * D])
    ada_b = din("ada_b", [6 * D])
    w_in = din("w_in", [D, 5632])
    hT_scr = dscr("hT_scr", [8, P, KC, 512], BF16)
    mod_scr = dscr("mod_scr", [6, D], F32)
    out_d = dscr("out", [NOWN, D], F32, out=True)

    cst = sb("cst", [P, C_END], F32)
    identb = sb("identb", [P, P], BF16)
    hmask = sb("hmask_s", [P, 1], F32)
    onesb = sb("onesb", [P, P], BF16)
    maskb = sb("maskb", [P, 256], BF16)
    maskh = sb("maskh", [P, P], BF16)
    pswapb = sb("pswapb", [P, P], BF16)
    slot_i = sb("slot_i", [P, 64], I32)
    gates = sb("gates", [P, 16, 4], F32)
    bchk = nc.gpsimd.alloc_register("bchk")
    nc.gpsimd.reg_mov(bchk, NSLOT - 1)
    idxs = [sb("idxs%d" % i, [P, 4], I32) for i in range(2)]
    nbt = sb("nbt", [1, NE], I32)
    nbf = sb("nbf", [1, NE], F32)
    scR = Scope(cx)
    cosT = scR.sb("cosT", [P, NALL], BF16)
    sinT = scR.sb("sinT", [P, NALL], BF16)
    scA = Scope(cx)
    sc1p = scA.sb("sc1p", [P, D], F32)
    sh1 = scA.sb("sh1", [P, D], F32)

    ps = [psum("ps%d" % i, [P, 512], F32) for i in range(8)]

    dma("sp", lambda q: q.dma_start(out=cst[:, :], in_=consts_d[:, :]), [consts_d], [cst])
    dma("sp", lambda q: q.dma_start(out=hmask[:, :], in_=hmask_d[:, :]), [hmask_d], [hmask])
    op("dve", lambda v: v.tensor_copy(identb[:, :], cst[:, C_ID:C_ID + 128]), [cst], [identb])

    if True:
        sc = Scope(cx)
        posi, ang, ang2 = sc.sb("posi", [P, NALL], I32), sc.sb("angf", [P, NALL], F32), sc.sb("ang2", [P, NALL], F32)
        dma("sp", lambda q: q.dma_start(out=posi[:, :], in_=posb.t.partition_broadcast(P)), [posb], [posi])
        op("dve", lambda v: v.tensor_copy(ang[:, :], posi[:, :]), [posi], [ang])
        op("dve", lambda v: v.tensor_scalar(ang[:, :], ang[:, :], cst[:, C_INVF:C_INVF + 1], None, ALU.mult),
           [ang, cst], [ang])
        op("dve", lambda v: v.tensor_scalar(ang2[:, :], ang[:, :], float(1.0 / TWO_PI), None, ALU.mult), [ang], [ang2])
        op("dve", lambda v: v.tensor_copy(posi[:, :], ang2[:, :]), [ang2], [posi])
        op("dve", lambda v: v.tensor_copy(ang2[:, :], posi[:, :]), [posi], [ang2])
        op("dve", lambda v: v.scalar_tensor_tensor(ang[:, :], ang2[:, :], -TWO_PI, ang[:, :], ALU.mult, ALU.add),
           [ang2, ang], [ang])
        op("dve", lambda v: v.tensor_scalar(ang2[:, :], ang[:, :], float(np.pi), -TWO_PI, ALU.is_gt, ALU.mult), [ang], [ang2])
        op("dve", lambda v: v.tensor_tensor(ang[:, :], ang[:, :], ang2[:, :], ALU.add), [ang, ang2], [ang])
        op("act", lambda a: a.activation(out=ang2[:, :], in_=ang[:, :], func=AF.Sin), [ang], [ang2])
        op("dve", lambda v: v.tensor_scalar(sinT[:, :], ang2[:, :], cst[:, C_SGN:C_SGN + 1], None, ALU.mult),
           [ang2, cst], [sinT])
        op("dve", lambda v: v.tensor_scalar_add(ang[:, :], ang[:, :], float(np.pi / 2)), [ang], [ang])
        op("dve", lambda v: v.tensor_scalar(ang2[:, :], ang[:, :], float(np.pi), -TWO_PI, ALU.is_gt, ALU.mult), [ang], [ang2])
        op("dve", lambda v: v.tensor_tensor(ang[:, :], ang[:, :], ang2[:, :], ALU.add), [ang, ang2], [ang])
        op("act", lambda a: a.activation(out=cosT[:, :], in_=ang[:, :], func=AF.Sin), [ang], [cosT])
        sc.close()

    if True:
        sc = Scope(cx)
        c_t, s_rep, abb, modt = sc.sb("c_t", [P, KC], F32), sc.sb("s_rep", [P, KC, P], F32), sc.sb("abb", [P, D], F32), sc.sb("modt", [P, D], F32)
        aw = [sc.sb("aw%d" % i, [P, D], F32) for i in range(3)]
        dma("sp", lambda q: q.dma_start(out=c_t[:, :], in_=cvec[:, :]), [cvec], [c_t])
        op("act", lambda a: a.activation(out=c_t[:, :], in_=c_t[:, :], func=AF.Silu), [c_t], [c_t])
        for kc in range(KC):
            op("dve", lambda v, kc=kc: v.tensor_copy(s_rep[:, kc, :], c_t[:, kc:kc + 1].to_broadcast([P, P])),
               [c_t], [s_rep])
        nld = 0
        for j in range(6):
            for kc in range(KC):
                b = aw[nld % 3]
                nld += 1
                dma("sp", lambda q, b=b, j=j, kc=kc: q.dma_start(out=b[:, :], in_=ada_w[kc * P:(kc + 1) * P, j * D:(j + 1) * D]),
                    [ada_w], [b])
                peg([lambda pe, b=b, n=n, kc=kc: pe.matmul(ps[n][:, :], s_rep[:, kc, :], b[:, n * 512:(n + 1) * 512],
                                                          start=(kc == 0), stop=(kc == KC - 1)) for n in range(4)],
                    [b, s_rep], [ps[0], ps[1], ps[2], ps[3]])
            dma("sp", lambda q, j=j: q.dma_start(out=abb[:, :], in_=ada_b.t[j * D:(j + 1) * D].partition_broadcast(P)),
                [ada_b], [abb])
            dst = sh1 if j == 0 else (sc1p if j == 1 else modt)
            for n in range(4):
                op("dve", lambda v, n=n, dst=dst: v.tensor_tensor(dst[:, n * 512:(n + 1) * 512], ps[n][:, :],
                                                                 abb[:, n * 512:(n + 1) * 512], ALU.add),
                   [ps[n], abb], [dst])
            if j == 1:
                op("dve", lambda v: v.tensor_scalar_add(sc1p[:, :], sc1p[:, :], 1.0), [sc1p], [sc1p])
            dma("sp", lambda q, j=j, dst=dst: q.dma_start(out=mod_scr[j:j + 1, :], in_=dst[0:1, :]), [dst], [mod_scr])
        sc.close()

    op("dve", lambda v: v.memset(onesb[:, :], 1.0), [], [onesb])
    op("dve", lambda v: v.tensor_copy(maskb[:, :], cst[:, C_MASK:C_MASK + 256]), [cst], [maskb])
    op("dve", lambda v: v.tensor_scalar(maskh[:, :], cst[:, C_MASK + 128:C_MASK + 256], hmask[:, 0:1], None, ALU.add),
       [cst, hmask], [maskh])
    op("dve", lambda v: v.tensor_copy(pswapb[:, :], cst[:, C_SWAP:C_SWAP + 128]), [cst], [pswapb])

    psn = [0]

    def nps():
        b = ps[psn[0] % 8]
        psn[0] += 1
        return b

    def bfview(pb, n=1024):
        return pb[:, :].bitcast(BF16)[:, 0:n]

    def ln_stats(src, st, mv, width=D):
        nchunk = width // 512
        for i in range(nchunk):
            op("dve", lambda v, i=i: v.bn_stats(st[:, i, :], src[:, i * 512:(i + 1) * 512]), [src], [st])
        op("dve", lambda v: v.bn_aggr(mv[:, 0:2], st[:, 0:nchunk, :]), [st], [mv])
        op("dve", lambda v: v.tensor_scalar_add(mv[:, 2:3], mv[:, 1:2], EPS), [mv], [mv])
        op("act", lambda a: a.activation(out=mv[:, 2:3], in_=mv[:, 2:3], func=AF.Sqrt), [mv], [mv])
        op("dve", lambda v: v.reciprocal(mv[:, 2:3], mv[:, 2:3]), [mv], [mv])
        op("dve", lambda v: v.tensor_scalar(mv[:, 3:4], mv[:, 0:1], mv[:, 2:3], -1.0, ALU.mult, ALU.mult), [mv], [mv])

    if True:
        sc = Scope(cx)
        xt = [sc.sb("xt%d" % i, [P, D], F32) for i in range(2)]
        hn = sc.sb("hn", [P, D], F32)
        hb = [sc.sb("hb%d" % i, [P, D], BF16) for i in range(2)]
        hTm = [sc.sb("hTm%d" % i, [P, KC, 512], BF16) for i in range(2)]
        st = sc.sb("st", [P, 4, 6], F32)
        mv = sc.sb("mv", [P, 4], F32)
        for lt in range(32):
            m, sub = lt // 4, lt % 4
            x_ = xt[lt % 2]
            h_ = hb[lt % 2]
            hT_ = hTm[m % 2]
            dma("sp", lambda q, x_=x_, lt=lt: q.dma_start(out=x_[:, :], in_=xin[lt * P:(lt + 1) * P, :]), [xin], [x_])
            ln_stats(x_, st, mv)
            op("act", lambda a, x_=x_: a.activation(out=hn[:, :], in_=x_[:, :], func=AF.Identity, bias=mv[:, 3:4], scale=mv[:, 2:3]),
               [x_, mv], [hn])
            op("pool", lambda g: g.tensor_tensor(hn[:, :], hn[:, :], sc1p[:, :], ALU.mult), [hn, sc1p], [hn])
            op("dve", lambda v, h_=h_: v.tensor_tensor(h_[:, :], hn[:, :], sh1[:, :], ALU.add), [hn, sh1], [h_])
            for half in range(2):
                pb = nps()
                peg([lambda pe, pb=pb, k=k, half=half, h_=h_: pe.transpose(bfview(pb)[:, k * P:(k + 1) * P],
                                                                        h_[:, (half * 8 + k) * P:(half * 8 + k + 1) * P], identb[:, :])
                     for k in range(8)], [h_, identb], [pb])
                op("act", lambda a, pb=pb, half=half, hT_=hT_, sub=sub: a.activation(
                    out=hT_[:, half * 8:(half + 1) * 8, sub * P:(sub + 1) * P],
                    in_=bfview(pb).rearrange("p (k t) -> p k t", t=P), func=AF.Identity), [pb], [hT_])
            if sub == 3:
                dma("sp", lambda q, hT_=hT_, m=m: q.dma_start(out=hT_scr[m, :, :, :], in_=hT_[:, :, :]), [hT_], [hT_scr])
        sc.close()
    scA.close()

    if stage == 1:
        cx.wait_all("sp", hT_scr)
        return nc

    wsT_d = din("wsT", [4, P, P])
    bspT_d = din("bspT", [P, 4])
    gln_g_d = din("gln_g", [512])
    gln_b_d = din("gln_b", [512])
    mixT_scr = dscr("mixT_scr", [4, P, 16, 512], BF16)
    w_in3 = w_in.t.rearrange("(kc p) n -> p kc n", p=P)

    numer_den_scope = None
    for g in range(NG):
        sc = Scope(cx)
        wg = sc.sb("wg", [P, KC, 3 * HG * P], BF16)
        QT = sc.sb("QT", [P, HG, NOWN], BF16)
        KT = sc.sb("KT", [P, HG, NALL], BF16)
        VT = sc.sb("VT", [P, HG, NALL], BF16)
        hTm = [sc.sb("hTg%d" % i, [P, KC, 512], BF16) for i in range(2)]
        rawb = [sc.sb("rawb%d" % i, [P, 512], BF16) for i in range(2)]
        t1 = [sc.sb("t1_%d" % i, [P, 512], F32) for i in range(2)]
        t2 = [sc.sb("t2_%d" % i, [P, 512], F32) for i in range(2)]
        W = HG * P
        for which in range(3):
            c0 = which * 1536 + g * W
            for kc in range(KC):
                dma("pool", lambda q, which=which, c0=c0, kc=kc: q.dma_start(out=wg[:, kc, which * W:(which + 1) * W],
                                                                              in_=w_in[kc * P:(kc + 1) * P, c0:c0 + W]), [w_in], [wg])
        un = 0
        for m in range(8):
            own = m >= 4
            hT_ = hTm[m % 2]
            dma("sp", lambda q, hT_=hT_, m=m: q.dma_start(out=hT_[:, :, :], in_=hT_scr[m, :, :, :]), [hT_scr], [hT_])
            tok0 = m * 512
            for hl in range(HG):
                for which in ((0, 1, 2) if own else (1, 2)):
                    if os.environ.get("KSKIP_QK") and which != 2:
                        continue
                    pb = nps()
                    cb = which * W + hl * P
                    peg([lambda pe, pb=pb, kc=kc, cb=cb, hT_=hT_: pe.matmul(pb[:, :], wg[:, kc, cb:cb + P], hT_[:, kc, :],
                                                                           start=(kc == 0), stop=(kc == KC - 1)) for kc in range(KC)],
                        [wg, hT_], [pb])
                    if which == 2:
                        op("act", lambda a, pb=pb, hl=hl, tok0=tok0: a.activation(out=VT[:, hl, tok0:tok0 + 512], in_=pb[:, :], func=AF.Identity),
                           [pb], [VT])
                        continue
                    rb, a1, a2 = rawb[un % 2], t1[un % 2], t2[un % 2]
                    un += 1
                    QKM = int(os.environ.get("QKM", "9"))
                    if which == 0:
                        dst, dl = QT, tok0 - NOWN
                    else:
                        dst, dl = KT, tok0
                    op("act", lambda a, pb=pb, rb=rb: a.activation(out=rb[:, :], in_=pb[:, :], func=AF.Identity), [pb], [rb])
                    if QKM == 1:
                        op("dve", lambda v, dst=dst, dl=dl, hl=hl, rb=rb: v.tensor_copy(dst[:, hl, dl:dl + 512], rb[:, :]), [rb], [dst])
                        continue
                    pb2 = nps()
                    peg([lambda pe, pb2=pb2, rb=rb: pe.matmul(pb2[:, :], pswapb[:, :], rb[:, :], start=True, stop=True)], [pswapb, rb], [pb2])
                    if QKM == 2:
                        op("dve", lambda v, dst=dst, dl=dl, hl=hl, pb2=pb2: v.tensor_copy(dst[:, hl, dl:dl + 512], pb2[:, :]), [pb2], [dst])
                        continue
                    op("dve", lambda v, pb=pb, a1=a1, tok0=tok0: v.tensor_tensor(a1[:, :], pb[:, :], cosT[:, tok0:tok0 + 512], ALU.mult),
                       [pb, cosT], [a1])
                    if QKM == 3:
                        op("dve", lambda v, dst=dst, dl=dl, hl=hl, a1=a1: v.tensor_copy(dst[:, hl, dl:dl + 512], a1[:, :]), [a1], [dst])
                        continue
                    op("dve", lambda v, pb2=pb2, a2=a2, tok0=tok0: v.tensor_tensor(a2[:, :], pb2[:, :], sinT[:, tok0:tok0 + 512], ALU.mult),
                       [pb2, sinT], [a2])
                    op(os.environ.get("KADD_ENG", "pool"), lambda gp, dst=dst, dl=dl, hl=hl, a1=a1, a2=a2: gp.tensor_tensor(dst[:, hl, dl:dl + 512], a1[:, :], a2[:, :], ALU.add),
                       [a1, a2], [dst])

        if stage in (1.5, 2) and dbg and g == 0:
            dq = dscr("dbg_q", [P, HG, NOWN], BF16)
            dk = dscr("dbg_k", [P, HG, NALL], BF16)
            dv = dscr("dbg_v", [P, HG, NALL], BF16)
            dma("sp", lambda q: q.dma_start(out=dq[:, :, :], in_=QT[:, :, :]), [QT], [dq])
            dma("sp", lambda q: q.dma_start(out=dk[:, :, :], in_=KT[:, :, :]), [KT], [dk])
            dma("sp", lambda q: q.dma_start(out=dv[:, :, :], in_=VT[:, :, :]), [VT], [dv])
            if stage == 1.5:
                for b_ in (dq, dk, dv):
                    cx.wait_all("sp", b_)
                return nc

        sq = sc.sb("sq", [P, NALL], BF16)
        kmx = sc.sb("kmx", [P, 16], F32)
        negb = sc.sb("negb", [1, NOWN], BF16)
        qn = sc.sb("qn", [1, 512], F32)
        numer = sc.sb("numer", [P, NOWN], F32)
        den = sc.sb("den", [P, NOWN], F32)
        attb = sc.sb("attb", [P, NOWN], BF16)
        PT = [sc.sb("PT%d" % i, [P, 256], BF16) for i in range(4)]
        vblk = [sc.sb("vblk%d" % i, [P, P], BF16) for i in range(4)]
        bO = [ps[0], ps[1]]
        bL = [ps[2], ps[3]]
        bS = [ps[4], ps[5]]
        bV = ps[6]
        bX = ps[7]
        for hl in range(HG):
            h = g * HG + hl
            op("act", lambda a, hl=hl: a.activation(out=sq[:, :], in_=KT[:, hl, :], func=AF.Square), [KT], [sq])
            for c in range(8):
                peg([lambda pe, c=c: pe.matmul(bX[:, :], onesb[:, :], sq[:, c * 512:(c + 1) * 512], start=True, stop=True)], [onesb, sq], [bX])
                op("dve", lambda v, c=c: v.tensor_reduce(kmx[:, c:c + 1], bX[:, :], AX.X, ALU.max), [bX], [kmx])
            op("dve", lambda v: v.tensor_reduce(kmx[:, 8:9], kmx[:, 0:8], AX.X, ALU.max), [kmx], [kmx])
            op("act", lambda a: a.activation(out=kmx[:, 9:10], in_=kmx[:, 8:9], func=AF.Sqrt), [kmx], [kmx])
            op("dve", lambda v: v.tensor_scalar(kmx[:, 9:10], kmx[:, 9:10], -1.0, None, ALU.mult), [kmx], [kmx])
            op("act", lambda a, hl=hl: a.activation(out=sq[:, 0:NOWN], in_=QT[:, hl, :], func=AF.Square), [QT], [sq])
            for c in range(4):
                peg([lambda pe, c=c: pe.matmul(bX[:, :], onesb[:, :], sq[:, c * 512:(c + 1) * 512], start=True, stop=True)], [onesb, sq], [bX])
                op("act", lambda a: a.activation(out=qn[0:1, :], in_=bX[0:1, :], func=AF.Sqrt), [bX], [qn])
                op("dve", lambda v, c=c: v.tensor_scalar(negb[0:1, c * 512:(c + 1) * 512], qn[0:1, :], kmx[0:1, 9:10], None, ALU.mult),
                   [qn, kmx], [negb])
            first_branch = True
            sidx = 0
            for d in (1, 4, 16):
                nb_own = 16 // d
                n0 = 16 // d
                for r in range(d):
                    for n in range(n0 - 1, n0 + nb_own):
                        halo = (n == n0 - 1)
                        last = (n == n0 + nb_own - 1)
                        ks = n * P * d + r
                        kAP = KT[:, hl, sl(ks, P, d)]
                        vAP = VT[:, hl, sl(ks, P, d)]
                        if halo:
                            q0, nq = ks + P * d - NOWN, P
                            mAP = maskh[:, :]
                        elif last:
                            q0, nq = ks - NOWN, P
                            mAP = maskb[:, 0:P]
                        else:
                            q0, nq = ks - NOWN, 2 * P
                            mAP = maskb[:, :]
                        qAP = QT[:, hl, sl(q0, nq, d)]
                        nbAP = negb[0:1, sl(q0, nq, d)]
                        S = bS[sidx % 2]
                        pt = PT[sidx % 4]
                        vb = vblk[sidx % 4]
                        sidx += 1
                        peg([lambda pe, S=S, kAP=kAP, qAP=qAP, nq=nq: pe.matmul(S[:, 0:nq], kAP, qAP, start=True, stop=False),
                             lambda pe, S=S, mAP=mAP, nq=nq: pe.matmul(S[:, 0:nq], identb[:, :], mAP, start=False, stop=False),
                             lambda pe, S=S, nbAP=nbAP, nq=nq: pe.matmul(S[:, 0:nq], onesb[0:1, :], nbAP, start=False, stop=True)],
                            [KT, QT, identb, maskb, maskh, onesb, negb], [S])
                        op("act", lambda a, S=S, pt=pt, nq=nq: a.activation(out=pt[:, 0:nq], in_=S[:, 0:nq], func=AF.Exp, scale=SM_SCALE),
                           [S], [pt])
                        peg([lambda pe, vAP=vAP: pe.transpose(bfview(bV)[:, 0:P], vAP, identb[:, :])], [VT, identb], [bV])
                        op("dve", lambda v, vb=vb: v.tensor_copy(vb[:, :], bfview(bV)[:, 0:P]), [bV], [vb])
                        served = []
                        if halo:
                            served.append((n + 1, 0, True))
                        elif last:
                            served.append((n, 0, False))
                        else:
                            served.append((n, 0, False))
                            served.append((n + 1, P, True))
                        for (qb, co, isfirst) in served:
                            O = bO[qb % 2]
                            L = bL[qb % 2]
                            peg([lambda pe, O=O, vb=vb, pt=pt, co=co, isfirst=isfirst: pe.matmul(
                                O[:, 0:P], vb[:, :], pt[:, co:co + P], start=isfirst, stop=(not isfirst))], [vb, pt], [O])
                            peg([lambda pe, L=L, pt=pt, co=co, isfirst=isfirst: pe.matmul(
                                L[:, 0:P], onesb[:, :], pt[:, co:co + P], start=isfirst, stop=(not isfirst))], [onesb, pt], [L])
                            if not isfirst:
                                qs = qb * P * d + r - NOWN
                                nAP = numer[:, sl(qs, P, d)]
                                dAP = den[:, sl(qs, P, d)]
                                if first_branch:
                                    op("dve", lambda v, O=O, nAP=nAP: v.tensor_copy(nAP, O[:, 0:P]), [O], [numer])
                                    op("dve", lambda v, L=L, dAP=dAP: v.tensor_copy(dAP, L[:, 0:P]), [L], [den])
                                else:
                                    op("dve", lambda v, O=O, nAP=nAP: v.tensor_tensor(nAP, O[:, 0:P], nAP, ALU.add), [O, numer], [numer])
                                    op("dve", lambda v, L=L, dAP=dAP: v.tensor_tensor(dAP, L[:, 0:P], dAP, ALU.add), [L, den], [den])
                first_branch = False
            op("dve", lambda v: v.reciprocal(den[:, :], den[:, :]), [den], [den])
            op("dve", lambda v: v.tensor_tensor(attb[:, :], numer[:, :], den[:, :], ALU.mult), [numer, den], [attb])
            for mo in range(4):
                dma("sp", lambda q, h=h, mo=mo: q.dma_start(out=mixT_scr[mo, :, h, :], in_=attb[:, mo * 512:(mo + 1) * 512]), [attb], [mixT_scr])
        sc.close()
        if stage == 2 and dbg and g == 0:
            cx.wait_all("sp", mixT_scr)
            return nc

    scR.close()
    if True:
        sc = Scope(cx)
        wu = sc.sb("wu", [P, KC, 512], BF16)
        wv = sc.sb("wv", [P, KC, 512], BF16)
        wsT = sc.sb("wsT_s", [P, 4, P], F32)
        wsTb = sc.sb("wsTb", [P, 4, P], BF16)
        bsp = sc.sb("bsp", [P, 4], F32)
        glg = sc.sb("glg", [P, 512], F32)
        glb = sc.sb("glb", [P, 512], F32)
        hTm = [sc.sb("hTu%d" % i, [P, KC, 512], BF16) for i in range(2)]
        ug = [sc.sb("ug%d" % i, [P, 512], F32) for i in range(2)]
        vg = [sc.sb("vg%d" % i, [P, 512], F32) for i in range(2)]
        vnb = [sc.sb("vnb%d" % i, [P, 512], BF16) for i in range(2)]
        gm = [sc.sb("gm%d" % i, [P, 512], BF16) for i in range(2)]
        gmT = sc.sb("gmT", [P, 4, NOWN], BF16)
        st = sc.sb("stg", [P, 4, 6], F32)
        mv = sc.sb("mvg", [P, 4], F32)
        for kc in range(KC):
            dma("pool", lambda q, kc=kc: q.dma_start(out=wu[:, kc, :], in_=w_in[kc * P:(kc + 1) * P, 4608:5120]), [w_in], [wu])
            dma("pool", lambda q, kc=kc: q.dma_start(out=wv[:, kc, :], in_=w_in[kc * P:(kc + 1) * P, 5120:5632]), [w_in], [wv])
        dma("sp", lambda q: q.dma_start(out=wsT[:, :, :], in_=wsT_d.t.rearrange("g s t -> s g t")), [wsT_d], [wsT])
        dma("sp", lambda q: q.dma_start(out=bsp[:, :], in_=bspT_d[:, :]), [bspT_d], [bsp])
        dma("sp", lambda q: q.dma_start(out=glg[:, :], in_=gln_g_d.t.partition_broadcast(P)), [gln_g_d], [glg])
        dma("sp", lambda q: q.dma_start(out=glb[:, :], in_=gln_b_d.t.partition_broadcast(P)), [gln_b_d], [glb])
        for gg in range(4):
            op("dve", lambda v, gg=gg: v.tensor_tensor(wsTb[:, gg, :], wsT[:, gg, :], cst[:, C_TRIU:C_TRIU + P], ALU.mult), [wsT, cst], [wsTb])
        ti = 0
        for m in range(4, 8):
            hT_ = hTm[m % 2]
            dma("sp", lambda q, hT_=hT_, m=m: q.dma_start(out=hT_[:, :, :], in_=hT_scr[m, :, :, :]), [hT_scr], [hT_])
            for sub in range(4):
                ot = (m - 4) * 4 + sub
                u_, v_, vn_, gm_ = ug[ti % 2], vg[ti % 2], vnb[ti % 2], gm[ti % 2]
                ti += 1
                pu, pv = nps(), nps()
                peg([lambda pe, pu=pu, kc=kc, hT_=hT_, sub=sub: pe.matmul(pu[:, :], hT_[:, kc, sub * P:(sub + 1) * P], wu[:, kc, :],
                                                                        start=(kc == 0), stop=(kc == KC - 1)) for kc in range(KC)], [hT_, wu], [pu])
                peg([lambda pe, pv=pv, kc=kc, hT_=hT_, sub=sub: pe.matmul(pv[:, :], hT_[:, kc, sub * P:(sub + 1) * P], wv[:, kc, :],
                                                                        start=(kc == 0), stop=(kc == KC - 1)) for kc in range(KC)], [hT_, wv], [pv])
                op("act", lambda a, pu=pu, u_=u_: a.activation(out=u_[:, :], in_=pu[:, :], func=AF.Gelu), [pu], [u_])
                op("act", lambda a, pv=pv, v_=v_: a.activation(out=v_[:, :], in_=pv[:, :], func=AF.Gelu), [pv], [v_])
                ln_stats(v_, st, mv, width=512)
                op("act", lambda a, v_=v_: a.activation(out=v_[:, :], in_=v_[:, :], func=AF.Identity, bias=mv[:, 3:4], scale=mv[:, 2:3]),
                   [v_, mv], [v_])
                op("dve", lambda v, v_=v_: v.tensor_tensor(v_[:, :], v_[:, :], glg[:, :], ALU.mult), [v_, glg], [v_])
                op("dve", lambda v, v_=v_, vn_=vn_: v.tensor_tensor(vn_[:, :], v_[:, :], glb[:, :], ALU.add), [v_, glb], [vn_])
                pq = nps()
                peg([lambda pe, pq=pq, gg=gg, vn_=vn_: pe.matmul(pq[:, gg * P:(gg + 1) * P], wsTb[:, gg, :], vn_[:, gg * P:(gg + 1) * P],
                                                               start=True, stop=True) for gg in range(4)], [wsTb, vn_], [pq])
                for gg in range(4):
                    op("dve", lambda v, pq=pq, gg=gg, u_=u_, gm_=gm_: v.scalar_tensor_tensor(
                        gm_[:, gg * P:(gg + 1) * P], pq[:, gg * P:(gg + 1) * P], bsp[:, gg:gg + 1], u_[:, gg * P:(gg + 1) * P], ALU.add, ALU.mult),
                       [pq, bsp, u_], [gm_])
                pt_ = nps()
                peg([lambda pe, pt_=pt_, gg=gg, gm_=gm_: pe.transpose(bfview(pt_)[:, gg * P:(gg + 1) * P], gm_[:, gg * P:(gg + 1) * P], identb[:, :])
                     for gg in range(4)], [gm_, identb], [pt_])
                op("act", lambda a, pt_=pt_, ot=ot: a.activation(out=gmT[:, :, ot * P:(ot + 1) * P],
                                                                in_=bfview(pt_, 512).rearrange("p (k t) -> p k t", t=P), func=AF.Identity),
                   [pt_], [gmT])
        for mo in range(4):
            dma("sp", lambda q, mo=mo: q.dma_start(out=mixT_scr[mo, :, 12:16, :], in_=gmT[:, :, mo * 512:(mo + 1) * 512]), [gmT], [mixT_scr])
        sc.close()

    if stage == 2:
        cx.wait_all("sp", mixT_scr)
        return nc

    w_out_d = din("w_out", [D, D])
    ln1_g_d = din("ln1_g", [D])
    ln1_b_d = din("ln1_b", [D])
    router_w_d = din("router_w", [D, NE])
    router_b_d = din("router_b", [NE])
    x1_scr = dscr("x1_scr", [NOWN, D], F32)
    Xs = dscr("Xs", [NSLOT, D], BF16)
    if True:
        sc = Scope(cx)
        wob = sc.sb("wob", [P, KC, D], BF16)
        g1bc = sc.sb("g1bc", [P, D], F32)
        l1g = sc.sb("l1g", [P, D], F32)
        l1b = sc.sb("l1b", [P, D], F32)
        sc2p = sc.sb("sc2p", [P, D], F32)
        sh2b = sc.sb("sh2b", [P, D], F32)
        mixTm = [sc.sb("mixTm%d" % i, [P, KC, 512], BF16) for i in range(2)]
        xt = [sc.sb("xt3_%d" % i, [P, D], F32) for i in range(2)]
        y1 = sc.sb("y1", [P, D], F32)
        x1t = sc.sb("x1t", [P, D], F32)
        h2b = sc.sb("h2b", [P, D], BF16)
        h2T = sc.sb("h2T", [P, KC, P], F32)
        rw = sc.sb("rw", [P, KC, NE], F32)
        rbb = sc.sb("rbb", [P, NE], F32)
        lg = sc.sb("lg", [P, NE], F32)
        m8 = sc.sb("m8", [P, 8], F32)
        sm = sc.sb("sm", [P, 8], F32)
        mkf = sc.sb("mkf", [P, NE], F32)
        mkb = sc.sb("mkb", [P, NE], BF16)
        ustrb = sc.sb("ustrb", [P, P], BF16)
        cntp = sc.sb("cntp", [P, NE], F32)
        slotf = sc.sb("slotf", [P, NE], F32)
        prod = sc.sb("prod", [P, NE], F32)
        slot4 = sc.sb("slot4", [P, 4], F32)
        st = sc.sb("st3", [P, 4, 6], F32)
        mv = sc.sb("mv3", [P, 4], F32)
        for kc in range(KC):
            dma("pool", lambda q, kc=kc: q.dma_start(out=wob[:, kc, :], in_=w_out_d[kc * P:(kc + 1) * P, :]), [w_out_d], [wob])
        dma("sp", lambda q: q.dma_start(out=g1bc[:, :], in_=mod_scr.t[2, :].partition_broadcast(P)), [mod_scr], [g1bc])
        dma("sp", lambda q: q.dma_start(out=sh2b[:, :], in_=mod_scr.t[3, :].partition_broadcast(P)), [mod_scr], [sh2b])
        dma("sp", lambda q: q.dma_start(out=sc2p[:, :], in_=mod_scr.t[4, :].partition_broadcast(P)), [mod_scr], [sc2p])
        dma("sp", lambda q: q.dma_start(out=l1g[:, :], in_=ln1_g_d.t.partition_broadcast(P)), [ln1_g_d], [l1g])
        dma("sp", lambda q: q.dma_start(out=l1b[:, :], in_=ln1_b_d.t.partition_broadcast(P)), [ln1_b_d], [l1b])
        dma("sp", lambda q: q.dma_start(out=rw[:, :, :], in_=router_w_d.t.rearrange("(kc p) e -> p kc e", p=P)), [router_w_d], [rw])
        dma("sp", lambda q: q.dma_start(out=rbb[:, :], in_=router_b_d.t.partition_broadcast(P)), [router_b_d], [rbb])
        op("dve", lambda v: v.tensor_scalar_add(sc2p[:, :], sc2p[:, :], 1.0), [sc2p], [sc2p])
        op("dve", lambda v: v.tensor_copy(ustrb[:, :], cst[:, C_USTR:C_USTR + P]), [cst], [ustrb])
        op("dve", lambda v: v.memset(cntp[:, :], 0.0), [], [cntp])
        for ot in range(16):
            m, sub = ot // 4, ot % 4
            mt_ = mixTm[m % 2]
            x_ = xt[ot % 2]
            if sub == 0:
                dma("sp", lambda q, mt_=mt_, m=m: q.dma_start(out=mt_[:, :, :], in_=mixT_scr[m, :, :, :]), [mixT_scr], [mt_])
            dma("sp", lambda q, x_=x_, ot=ot: q.dma_start(out=x_[:, :], in_=xin[NOWN + ot * P:NOWN + (ot + 1) * P, :]), [xin], [x_])
            for n in range(4):
                pb = nps()
                peg([lambda pe, pb=pb, c=c, n=n, mt_=mt_, sub=sub: pe.matmul(pb[:, :], mt_[:, c, sub * P:(sub + 1) * P], wob[:, c, n * 512:(n + 1) * 512],
                                                                          start=(c == 0), stop=(c == KC - 1)) for c in range(KC)], [mt_, wob], [pb])
                op("dve", lambda v, pb=pb, n=n: v.tensor_tensor(y1[:, n * 512:(n + 1) * 512], pb[:, :], g1bc[:, n * 512:(n + 1) * 512], ALU.mult),
                   [pb, g1bc], [y1])
            op("dve", lambda gp, x_=x_: gp.scalar_tensor_tensor(y1[:, :], x_[:, :], ALPHA, y1[:, :], ALU.mult, ALU.add), [x_, y1], [y1])
            ln_stats(y1, st, mv)
            op("act", lambda a: a.activation(out=x1t[:, :], in_=y1[:, :], func=AF.Identity, bias=mv[:, 3:4], scale=mv[:, 2:3]), [y1, mv], [x1t])
            op("dve", lambda v: v.tensor_tensor(x1t[:, :], x1t[:, :], l1g[:, :], ALU.mult), [x1t, l1g], [x1t])
            op("pool", lambda gp: gp.tensor_tensor(x1t[:, :], x1t[:, :], l1b[:, :], ALU.add), [x1t, l1b], [x1t])
            dma("sp", lambda q, ot=ot: q.dma_start(out=x1_scr[ot * P:(ot + 1) * P, :], in_=x1t[:, :]), [x1t], [x1_scr])
            ln_stats(x1t, st, mv)
            op("act", lambda a: a.activation(out=y1[:, :], in_=x1t[:, :], func=AF.Identity, bias=mv[:, 3:4], scale=mv[:, 2:3]), [x1t, mv], [y1])
            op("dve", lambda v: v.tensor_tensor(y1[:, :], y1[:, :], sc2p[:, :], ALU.mult), [y1, sc2p], [y1])
            op("pool", lambda gp: gp.tensor_tensor(y1[:, :], y1[:, :], sh2b[:, :], ALU.add), [y1, sh2b], [y1])
            op("act", lambda a: a.activation(out=h2b[:, :], in_=y1[:, :], func=AF.Identity), [y1], [h2b])
            for qd in range(4):
                pb = nps()
                peg([lambda pe, pb=pb, k=k, qd=qd: pe.transpose(pb[:, k * P:(k + 1) * P], y1[:, (qd * 4 + k) * P:(qd * 4 + k + 1) * P], cst[:, C_ID:C_ID + P])
                     for k in range(4)], [y1, cst], [pb])
                op("act", lambda a, pb=pb, qd=qd: a.activation(out=h2T[:, qd * 4:(qd + 1) * 4, :], in_=pb[:, :].rearrange("p (k t) -> p k t", t=P),
                                                            func=AF.Identity), [pb], [h2T])
            pl = nps()
            peg([lambda pe, pl=pl, kc=kc: pe.matmul(pl[:, 0:NE], h2T[:, kc, :], rw[:, kc, :], start=(kc == 0), stop=(kc == KC - 1)) for kc in range(KC)],
                [h2T, rw], [pl])
            op("dve", lambda v, pl=pl: v.tensor_tensor(lg[:, :], pl[:, 0:NE], rbb[:, :], ALU.add), [pl, rbb], [lg])
            op("dve", lambda v: v.max(m8[:, :], lg[:, :]), [lg], [m8])
            op("dve", lambda v: v.tensor_scalar(sm[:, 0:1], m8[:, 0:1], -1.0, None, ALU.mult), [m8], [sm])
            op("act", lambda a: a.activation(out=sm[:, 4:8], in_=m8[:, 0:4], func=AF.Exp, bias=sm[:, 0:1], scale=1.0), [m8, sm], [sm])
            op("dve", lambda v: v.tensor_reduce(sm[:, 1:2], sm[:, 4:8], AX.X, ALU.add), [sm], [sm])
            op("dve", lambda v: v.reciprocal(sm[:, 2:3], sm[:, 1:2]), [sm], [sm])
            op("dve", lambda v, ot=ot: v.tensor_scalar(gates[:, ot, :], sm[:, 4:8], sm[:, 2:3], None, ALU.mult), [sm], [gates])
            op("dve", lambda v: v.tensor_scalar(mkf[:, :], lg[:, :], m8[:, 3:4], None, ALU.is_ge), [lg, m8], [mkf])
            op("dve", lambda v: v.tensor_copy(mkb[:, :], mkf[:, :]), [mkf], [mkb])
            pr = nps()
            peg([lambda pe, pr=pr: pe.matmul(pr[:, 0:NE], ustrb[:, :], mkb[:, :], start=True, stop=True),
                 lambda pe, pr=pr: pe.matmul(pr[:, NE:2 * NE], onesb[:, :], mkb[:, :], start=True, stop=True)], [ustrb, onesb, mkb], [pr])
            op("dve", lambda v, pr=pr: v.tensor_tensor(slotf[:, :], pr[:, 0:NE], cntp[:, :], ALU.add), [pr, cntp], [slotf])
            op("dve", lambda v: v.tensor_scalar(prod[:, :], slotf[:, :], float(CAP), 1.0e6, ALU.is_ge, ALU.mult), [slotf], [prod])
            op("dve", lambda v: v.tensor_tensor(slotf[:, :], slotf[:, :], cst[:, C_EOFF:C_EOFF + NE], ALU.add), [slotf, cst], [slotf])
            op("dve", lambda v: v.tensor_tensor(slotf[:, :], slotf[:, :], prod[:, :], ALU.add), [slotf, prod], [slotf])
            op("dve", lambda v, pr=pr: v.tensor_tensor(cntp[:, :], pr[:, NE:2 * NE], cntp[:, :], ALU.add), [pr, cntp], [cntp])
            for k in range(4):
                op("dve", lambda v, k=k: v.scalar_tensor_tensor(prod[:, :], lg[:, :], m8[:, k:k + 1], slotf[:, :], ALU.is_equal, ALU.mult),
                   [lg, m8, slotf], [prod])
                op("dve", lambda v, k=k: v.tensor_reduce(slot4[:, k:k + 1], prod[:, :], AX.X, ALU.add), [prod], [slot4])
            op("dve", lambda v, ot=ot: v.tensor_copy(slot_i[:, ot * 4:ot * 4 + 4], slot4[:, :]), [slot4], [slot_i])
            op("dve", lambda v: v.tensor_scalar(sm[:, 4:8], slot4[:, :], float(NSLOT), None, ALU.is_lt), [slot4], [sm])
            op("dve", lambda v, ot=ot: v.tensor_tensor(gates[:, ot, :], gates[:, ot, :], sm[:, 4:8], ALU.mult), [gates, sm], [gates])
            ix = idxs[ot % 2]
            op("dve", lambda v, ix=ix: v.tensor_copy(ix[:, :], slot4[:, :]), [slot4], [ix])
            for k in range(4):
                dma("pool", lambda q, ix=ix, k=k: q.indirect_dma_start(
                    out=Xs[:, :], out_offset=bass.IndirectOffsetOnAxis(ap=ix[:, k:k + 1], axis=0),
                    in_=h2b[:, :], in_offset=None, bounds_check=bchk, oob_is_err=False), [h2b, ix], [Xs])
        op("dve", lambda v: v.tensor_scalar(nbf[0:1, :], cntp[0:1, :], 127.0, 1.0 / 128.0, ALU.add, ALU.mult), [cntp], [nbf])
        op("dve", lambda v: v.tensor_scalar(nbf[0:1, :], nbf[0:1, :], -0.496, float(CAP // P), ALU.add, ALU.min), [nbf], [nbf])
        op("dve", lambda v: v.tensor_copy(nbt[0:1, :], nbf[0:1, :]), [nbf], [nbt])
        sc.close()

    if stage == 3:
        dbg3 = dscr("dbg3", [P, 16, 8], F32, out=True)
        if True:
            sc = Scope(cx)
            tmp = sc.sb("dbgtmp", [P, 16, 8], F32)
            op("dve", lambda v: v.tensor_copy(tmp[:, :, 0:4], slot_i[:, :].rearrange("p (t k) -> p t k", k=4)), [slot_i], [tmp])
            op("dve", lambda v: v.tensor_copy(tmp[:, :, 4:8], gates[:, :, :]), [gates], [tmp])
            dma("sp", lambda q: q.dma_start(out=dbg3[:, :, :], in_=tmp[:, :, :]), [tmp], [dbg3])
            cx.wait_all("sp", dbg3)
            cx.wait_all("sp", x1_scr)
            cx.wait_all("sp", Xs)
            sc.close()
        return nc

    w_gu_d = din("w_gu", [NE, D, 2 * D])
    w_d_d = din("w_d", [NE, D, D])
    bguT_d = din("bguT", [NE, P, 32])
    b_d_d = din("b_d", [NE, D])
    ln2_g_d = din("ln2_g", [D])
    ln2_b_d = din("ln2_b", [D])
    Y = dscr("Y", [NSLOT, D], F32)
    NSB = CAP // P
    NHALF = CAP // 512
    if True:
        sc = Scope(cx)
        stg = [sc.sb("stg%d" % i, [P, 8, 512], F32) for i in range(2)]
        wbf = [sc.sb("wbf%d" % i, [P, KC, 512], BF16) for i in range(3)]
        xrow = [sc.sb("xrow%d" % i, [P, D], BF16) for i in range(2)]
        xT = sc.sb("xT", [P, KC, CAP], BF16)
        actT = sc.sb("actT", [P, KC, CAP], BF16)
        yst = [sc.sb("yst%d" % i, [P, 512], F32) for i in range(3)]
        bdbc = [sc.sb("bdbc%d" % i, [P, D], BF16) for i in range(2)]
        bgu = [sc.sb("bgu%d" % i, [P, 32], F32) for i in range(2)]
        gt = [sc.sb("gt%d" % i, [P, 512], F32) for i in range(2)]
        ut = [sc.sb("ut%d" % i, [P, 512], F32) for i in range(2)]
        sg = [sc.sb("sg%d" % i, [P, 512], F32) for i in range(2)]
        NW = len(wbf)

        gran = []
        for e in range(NE):
            for q4 in range(4):
                gran.append((e, 0, q4))
                gran.append((e, 1, q4))
            for n in range(4):
                gran.append((e, 2, n))
        emitted = [0]
        cast_rot = ["dve", "act", "dve", "pool"]
        hcount = [0]

        def emit_load(i):
            e, kind, idx = gran[i]
            wb = wbf[i % NW]
            for half in range(2):
                s_ = stg[hcount[0] % len(stg)]
                eng = cast_rot[hcount[0] % 4]
                hcount[0] += 1
                if kind == 2:
                    src = w_d_d.t[e].rearrange("(kc p) n -> p kc n", p=P)[:, half * 8:(half + 1) * 8, idx * 512:(idx + 1) * 512]
                    dep = w_d_d
                else:
                    c0 = kind * D + idx * 512
                    src = w_gu_d.t[e].rearrange("(kc p) n -> p kc n", p=P)[:, half * 8:(half + 1) * 8, c0:c0 + 512]
                    dep = w_gu_d
                dma("sp", lambda q, s_=s_, src=src: q.dma_start(out=s_[:, :, :], in_=src), [dep], [s_])
                if eng == "act":
                    op("act", lambda a, wb=wb, half=half, s_=s_: a.activation(out=wb[:, half * 8:(half + 1) * 8, :], in_=s_[:, :, :], func=AF.Identity),
                       [s_], [wb])
                else:
                    op(eng, lambda v, wb=wb, half=half, s_=s_: v.tensor_copy(wb[:, half * 8:(half + 1) * 8, :], s_[:, :, :]), [s_], [wb])

        def ensure(upto):
            while emitted[0] < min(upto, len(gran)):
                emit_load(emitted[0])
                emitted[0] += 1

        for en in ("pe", "act", "dve", "pool", "sp"):
            cx.regs[en] = cx.E[en].alloc_register("nb_" + en)
        gi = 0
        ensure(2)
        cnt2 = 0
        ycnt = 0
        for e in range(NE):
            bg_, bd_ = bgu[e % 2], bdbc[e % 2]
            for en in ("pe", "act", "dve", "pool", "sp"):
                cx.wait_all(en, nbt)
                cx.E[en].reg_load(cx.regs[en], nbt[0:1, e:e + 1])
            dma("sp", lambda q, e=e, bg_=bg_: q.dma_start(out=bg_[:, :], in_=bguT_d[e, :, :]), [bguT_d], [bg_])
            dma("pool", lambda q, e=e, bd_=bd_: q.dma_start(out=bd_[:, :], in_=b_d_d.t[e, :].partition_broadcast(P)), [b_d_d], [bd_])
            for sbk in range(NSB):
                with cx.region(("sp", "pe", "act"), sbk + 1):
                    xr = xrow[(e * NSB + sbk) % 2]
                    r0 = e * CAP + sbk * P
                    dma("sp", lambda q, xr=xr, r0=r0: q.dma_start(out=xr[:, :], in_=Xs[r0:r0 + P, :]), [Xs], [xr])
                    for half in range(2):
                        pb = nps()
                        peg([lambda pe, pb=pb, k=k, half=half, xr=xr: pe.transpose(bfview(pb)[:, k * P:(k + 1) * P],
                                                                                 xr[:, (half * 8 + k) * P:(half * 8 + k + 1) * P], identb[:, :])
                             for k in range(8)], [xr, identb], [pb])
                        op("act", lambda a, pb=pb, half=half, sbk=sbk: a.activation(
                            out=xT[:, half * 8:(half + 1) * 8, sbk * P:(sbk + 1) * P],
                            in_=bfview(pb).rearrange("p (k t) -> p k t", t=P), func=AF.Identity), [pb], [xT])
            for q4 in range(4):
                ensure(gi + 3)
                wg_, wu_ = wbf[gi % NW], wbf[(gi + 1) % NW]
                for jj in range(4):
                    j = q4 * 4 + jj
                    for qt in range(CAP // 256):
                        with cx.region(("pe", "dve", "pool", "act"), 2 * qt + 1):
                            g_, u_, s_g = gt[cnt2 % 2], ut[cnt2 % 2], sg[cnt2 % 2]
                            cnt2 += 1
                            pg, pu = nps(), nps()
                            c0 = qt * 256
                            peg([lambda pe, pg=pg, kc=kc, jj=jj, wg_=wg_, c0=c0: pe.matmul(pg[:, 0:256], wg_[:, kc, jj * P:(jj + 1) * P], xT[:, kc, c0:c0 + 256],
                                                                                          start=(kc == 0), stop=(kc == KC - 1)) for kc in range(KC)], [wg_, xT], [pg])
                            peg([lambda pe, pu=pu, kc=kc, jj=jj, wu_=wu_, c0=c0: pe.matmul(pu[:, 0:256], wu_[:, kc, jj * P:(jj + 1) * P], xT[:, kc, c0:c0 + 256],
                                                                                          start=(kc == 0), stop=(kc == KC - 1)) for kc in range(KC)], [wu_, xT], [pu])
                            op("dve", lambda v, pg=pg, g_=g_, j=j, bg_=bg_: v.tensor_scalar(g_[:, 0:256], pg[:, 0:256], bg_[:, j:j + 1], 7.0, ALU.add, ALU.min),
                               [pg, bg_], [g_])
                            op("dve", lambda v, pu=pu, u_=u_, j=j, bg_=bg_: v.tensor_scalar(u_[:, 0:256], pu[:, 0:256], bg_[:, 16 + j:17 + j], 7.0, ALU.add, ALU.min),
                               [pu, bg_], [u_])
                            op("pool", lambda gp, u_=u_: gp.tensor_scalar(u_[:, 0:256], u_[:, 0:256], -7.0, 1.0, ALU.max, ALU.add), [u_], [u_])
                            op("act", lambda a, g_=g_, s_g=s_g: a.activation(out=s_g[:, 0:256], in_=g_[:, 0:256], func=AF.Sigmoid, scale=1.702), [g_], [s_g])
                            op("pool", lambda gp, g_=g_, s_g=s_g: gp.tensor_tensor(g_[:, 0:256], g_[:, 0:256], s_g[:, 0:256], ALU.mult), [g_, s_g], [g_])
                            op("dve", lambda v, g_=g_, u_=u_, j=j, c0=c0: v.tensor_tensor(actT[:, j, c0:c0 + 256], g_[:, 0:256], u_[:, 0:256], ALU.mult), [g_, u_], [actT])
                gi += 2
            for n in range(4):
                ensure(gi + 2)
                wd_ = wbf[gi % NW]
                for sbk in range(NSB):
                    with cx.region(("pe", "dve", "sp"), sbk + 1):
                        ys = yst[ycnt % 3]
                        ycnt += 1
                        pb = nps()
                        peg([lambda pe, pb=pb, j=j, sbk=sbk, wd_=wd_: pe.matmul(pb[:, :], actT[:, j, sbk * P:(sbk + 1) * P], wd_[:, j, :],
                                                                               start=(j == 0), stop=(j == KC - 1)) for j in range(KC)], [actT, wd_], [pb])
                        op("dve", lambda v, pb=pb, ys=ys, n=n, bd_=bd_: v.tensor_tensor(ys[:, :], pb[:, :], bd_[:, n * 512:(n + 1) * 512], ALU.add),
                           [pb, bd_], [ys])
                        r0 = e * CAP + sbk * P
                        dma("sp", lambda q, ys=ys, r0=r0, n=n: q.dma_start(out=Y[r0:r0 + P, n * 512:(n + 1) * 512], in_=ys[:, :]), [ys], [Y])
                gi += 1
        sc.close()

    if True:
        sc = Scope(cx)
        g2bc = sc.sb("g2bc", [P, D], F32)
        l2g = sc.sb("l2g", [P, D], F32)
        l2b = sc.sb("l2b", [P, D], F32)
        yk = [sc.sb("yk%d" % i, [P, D], F32) for i in range(4)]
        x1r = [sc.sb("x1r%d" % i, [P, D], F32) for i in range(2)]
        acc = sc.sb("acc", [P, D], F32)
        ot_ = [sc.sb("outt%d" % i, [P, D], F32) for i in range(2)]
        st = sc.sb("st4", [P, 4, 6], F32)
        mv = sc.sb("mv4", [P, 4], F32)
        for k in range(4):
            op("dve", lambda v, k=k: v.memset(yk[k][:, :], 0.0), [], [yk[k]])
        dma("sp", lambda q: q.dma_start(out=g2bc[:, :], in_=mod_scr.t[5, :].partition_broadcast(P)), [mod_scr], [g2bc])
        dma("sp", lambda q: q.dma_start(out=l2g[:, :], in_=ln2_g_d.t.partition_broadcast(P)), [ln2_g_d], [l2g])
        dma("sp", lambda q: q.dma_start(out=l2b[:, :], in_=ln2_b_d.t.partition_broadcast(P)), [ln2_b_d], [l2b])
        for ot in range(16):
            x1_ = x1r[ot % 2]
            o_ = ot_[ot % 2]
            dma("sp", lambda q, x1_=x1_, ot=ot: q.dma_start(out=x1_[:, :], in_=x1_scr[ot * P:(ot + 1) * P, :]), [x1_scr], [x1_])
            ix = idxs[ot % 2]
            op("dve", lambda v, ix=ix, ot=ot: v.tensor_copy(ix[:, :], slot_i[:, ot * 4:ot * 4 + 4]), [slot_i], [ix])
            for k in range(4):
                dma("pool", lambda q, ix=ix, k=k: q.indirect_dma_start(
                    out=yk[k][:, :], out_offset=None, in_=Y[:, :],
                    in_offset=bass.IndirectOffsetOnAxis(ap=ix[:, k:k + 1], axis=0), bounds_check=bchk, oob_is_err=False),
                    [Y, ix], [yk[k]])
            op("dve", lambda v, ot=ot: v.tensor_scalar(acc[:, :], yk[0][:, :], gates[:, ot, 0:1], None, ALU.mult), [yk[0], gates], [acc])
            for k in range(1, 4):
                op("dve", lambda v, ot=ot, k=k: v.scalar_tensor_tensor(acc[:, :], yk[k][:, :], gates[:, ot, k:k + 1], acc[:, :], ALU.mult, ALU.add),
                   [yk[k], gates, acc], [acc])
            op("pool", lambda gp: gp.tensor_tensor(acc[:, :], acc[:, :], g2bc[:, :], ALU.mult), [acc, g2bc], [acc])
            op("dve", lambda v, x1_=x1_: v.scalar_tensor_tensor(acc[:, :], x1_[:, :], ALPHA, acc[:, :], ALU.mult, ALU.add), [x1_, acc], [acc])
            ln_stats(acc, st, mv)
            op("act", lambda a, o_=o_: a.activation(out=o_[:, :], in_=acc[:, :], func=AF.Identity, bias=mv[:, 3:4], scale=mv[:, 2:3]), [acc, mv], [o_])
            op("pool", lambda gp, o_=o_: gp.tensor_tensor(o_[:, :], o_[:, :], l2g[:, :], ALU.mult), [o_, l2g], [o_])
            op("dve", lambda v, o_=o_: v.tensor_tensor(o_[:, :], o_[:, :], l2b[:, :], ALU.add), [o_, l2b], [o_])
            dma("sp", lambda q, o_=o_, ot=ot: q.dma_start(out=out_d[ot * P:(ot + 1) * P, :], in_=o_[:, :]), [o_], [out_d])
        cx.wait_all("sp", out_d)
        sc.close()
    return nc


def make_in_maps(inputs, stage=99):
    x = np.asarray(inputs["x"], np.float32)
    c = np.asarray(inputs["c"], np.float32)
    pos = np.asarray(inputs["positions"], np.int32)
    consts = make_consts()
    maps = []
    for core in range(8):
        b, half = core // 2, core % 2
        m = {}
        if half == 0:
            xin = np.concatenate([np.zeros((NOWN, D), np.float32), x[b, 0:NOWN]], axis=0)
            pp = np.concatenate([np.zeros((NOWN,), np.int32), pos[b, 0:NOWN]])
            hm = np.full((P, 1), -BIG, np.float32)
        else:
            xin = x[b]
            pp = pos[b]
            hm = np.zeros((P, 1), np.float32)
        m["xin"] = np.ascontiguousarray(xin)
        m["posb"] = np.ascontiguousarray(pp)
        m["cvec"] = np.ascontiguousarray(c[b].reshape(KC, P).T)
        m["hmask"] = hm
        m["consts"] = consts
        m["ada_w"] = np.asarray(inputs["ada_w"], np.float32)[0]
        m["ada_b"] = np.asarray(inputs["ada_b"], np.float32)[0]
        m["w_in"] = np.asarray(inputs["w_in"], np.float32)[0]
        if stage >= 2:
            m["wsT"] = np.ascontiguousarray(np.asarray(inputs["w_spatial"], np.float32)[0].transpose(0, 2, 1))
            m["bspT"] = np.ascontiguousarray(np.asarray(inputs["b_spatial"], np.float32)[0].T)
            m["gln_g"] = np.asarray(inputs["gmlp_ln_g"], np.float32)[0]
            m["gln_b"] = np.asarray(inputs["gmlp_ln_b"], np.float32)[0]
        if stage >= 3:
            m["w_out"] = np.asarray(inputs["w_out"], np.float32)[0]
            m["ln1_g"] = np.asarray(inputs["ln1_g"], np.float32)[0]
            m["ln1_b"] = np.asarray(inputs["ln1_b"], np.float32)[0]
            m["router_w"] = np.asarray(inputs["router_w"], np.float32)[0]
            m["router_b"] = np.asarray(inputs["router_b"], np.float32)[0]
        if stage >= 4:
            m["w_gu"] = np.asarray(inputs["w_gate_up"], np.float32)[0]
            m["w_d"] = np.asarray(inputs["w_down"], np.float32)[0]
            bgu = np.asarray(inputs["b_gate_up"], np.float32)[0]
            m["bguT"] = np.ascontiguousarray(bgu.reshape(NE, 32, P).transpose(0, 2, 1))
            m["b_d"] = np.asarray(inputs["b_down"], np.float32)[0]
            m["ln2_g"] = np.asarray(inputs["ln2_g"], np.float32)[0]
            m["ln2_b"] = np.asarray(inputs["ln2_b"], np.float32)[0]
        maps.append(m)
    return maps


def kernel(**inputs):
    nc = build()
    maps = make_in_maps(inputs)
    res = run_bass_kernel_spmd(nc, maps, core_ids=list(range(8)))
    out = np.zeros((4, 4096, D), np.float32)
    for core in range(8):
        b, half = core // 2, core % 2
        out[b, half * NOWN:(half + 1) * NOWN] = res.results[core]["out"]
    return out
```

```python
import os
import numpy as np
from contextlib import ExitStack
import concourse.bass as bass
import concourse.mybir as mybir
from concourse.bass_utils import run_bass_kernel_spmd

F32 = mybir.dt.float32
BF16 = mybir.dt.bfloat16
I32 = mybir.dt.int32
AF = mybir.ActivationFunctionType
ALU = mybir.AluOpType
AX = mybir.AxisListType

P = 128
D = 2048
KC = 16
NOWN = 2048
NALL = 4096
NH = 12
HG = 3
NG = NH // HG
NE = 32
CAP = 1024
NSLOT = NE * CAP
ALPHA = float(2.0 ** 0.25)
EPS = 1e-5
BIG = 30000.0
SM_SCALE = float(1.0 / np.sqrt(128.0))
TWO_PI = float(2.0 * np.pi)

C_ID, C_MASK, C_SWAP, C_TRIU, C_USTR, C_EOFF, C_INVF, C_SGN, C_END = 0, 128, 384, 512, 640, 768, 800, 801, 802


def make_consts():
    c = np.zeros((128, C_END), np.float32)
    i = np.arange(128)
    c[:, C_ID:C_ID + 128] = np.eye(128, dtype=np.float32)
    k = i[:, None]
    q = i[None, :]
    c[:, C_MASK:C_MASK + 128] = np.where(k <= q, 0.0, -BIG)
    c[:, C_MASK + 128:C_MASK + 256] = np.where(k >= q, 0.0, -BIG)
    sw = np.zeros((128, 128), np.float32)
    sw[(i + 64) % 128, i] = 1.0
    c[:, C_SWAP:C_SWAP + 128] = sw
    c[:, C_TRIU:C_TRIU + 128] = (k <= q).astype(np.float32)
    c[:, C_USTR:C_USTR + 128] = (k < q).astype(np.float32)
    c[:, C_EOFF:C_EOFF + 32] = (np.arange(32) * CAP)[None, :].astype(np.float32)
    invf = np.power(np.float32(10000.0), -np.arange(64, dtype=np.float32) * np.float32(2.0 / 128)).astype(np.float32)
    c[:, C_INVF] = invf[i % 64]
    c[:, C_SGN] = np.where(i < 64, -1.0, 1.0)
    return c


def sl(start, n, step):
    return slice(start, start + (n - 1) * step + 1, step)


class Buf:
    def __init__(self, t):
        self.t = t
        self.w = {}
        self.r = {}
        self.excl = False

    def __getitem__(self, k):
        return self.t[k]


class Ctx:
    def __init__(self, nc):
        self.nc = nc
        self.E = dict(pe=nc.tensor, act=nc.scalar, dve=nc.vector, pool=nc.gpsimd, sp=nc.sync)
        self.csem = {}
        self.ccnt = {}
        for e in ("pe", "act", "dve", "pool"):
            self.csem[e] = nc.alloc_semaphore(name="c_" + e)
            self.ccnt[e] = 0
        self.waited = {e: {} for e in self.E}
        self.dsems = {}
        self.dcnt = {}
        self.dnext = {}
        for q, n in (("sp", 12), ("pool", 10), ("act", 8)):
            self.dsems[q] = [nc.alloc_semaphore(name="d_%s%d" % (q, i)) for i in range(n)]
            self.dnext[q] = 0
        self.semname = {}
        self.rec = None
        self.regs = {}

    def _key(self, sem):
        return id(sem)

    def wait_all(self, e, b):
        for t in list(b.w.values()):
            self.wait(e, t)

    def wait(self, e, tok):
        if tok is None:
            return
        sem, val = tok
        if e == "pe" and sem is self.csem["pe"]:
            return
        k = self._key(sem)
        if self.waited[e].get(k, 0) >= val:
            return
        if self.rec is not None:
            self.rec[e].append(lambda E=self.E[e], sem=sem, val=val: E.wait_ge(sem, val))
        else:
            self.E[e].wait_ge(sem, val)
        self.waited[e][k] = val

    @staticmethod
    def _split(reads, writes):
        writes = list(writes) + [b for b in reads if b.excl and b not in writes]
        reads = [b for b in reads if not b.excl]
        return reads, writes

    def _deps(self, e, reads, writes):
        for b in reads:
            for t in list(b.w.values()):
                self.wait(e, t)
        for b in writes:
            for t in list(b.w.values()):
                self.wait(e, t)
            for t in list(b.r.values()):
                self.wait(e, t)

    def _commit(self, tok, reads, writes):
        for b in reads:
            k = self._key(tok[0])
            old = b.r.get(k)
            if old is None or old[1] < tok[1]:
                b.r[k] = tok
        for b in writes:
            k = self._key(tok[0])
            if tok[0] in self.csem.values():
                b.w = {k: tok}
            else:
                b.w[k] = tok
            b.r = {}

    def release(self, bufs):
        for e in ("pe", "act", "dve", "pool", "sp"):
            for b in bufs:
                for t in list(b.w.values()):
                    self.wait(e, t)
                for t in list(b.r.values()):
                    self.wait(e, t)

    def op(self, e, fn, reads=(), writes=()):
        reads, writes = self._split(reads, writes)
        self._deps(e, reads, writes)
        self.ccnt[e] += 1
        if self.rec is not None:
            self.rec[e].append(lambda E=self.E[e], fn=fn, sem=self.csem[e]: fn(E).then_inc(sem, 1))
        else:
            fn(self.E[e]).then_inc(self.csem[e], 1)
        tok = (self.csem[e], self.ccnt[e])
        self._commit(tok, reads, writes)
        return tok

    def region(self, engines, thr):
        return _Region(self, engines, thr)

    def pe_group(self, fns, reads=(), writes=()):
        reads, writes = self._split(reads, writes)
        self._deps("pe", reads, writes)
        self.ccnt["pe"] += 1

        def emit(E=self.E["pe"], fns=list(fns), sem=self.csem["pe"]):
            inst = None
            for fn in fns:
                inst = fn(E)
            inst.then_inc(sem, 1)
        if self.rec is not None:
            self.rec["pe"].append(emit)
        else:
            emit()
        tok = (self.csem["pe"], self.ccnt["pe"])
        self._commit(tok, reads, writes)
        return tok

    def dma(self, q, fn, reads=(), writes=()):
        sems = self.dsems[q]
        s = sems[self.dnext[q] % len(sems)]
        self.dnext[q] += 1
        k = self._key(s)
        n = self.dcnt.get(k, 0)
        if n > 0:
            self.wait(q, (s, 16 * n))
        self._deps(q, reads, writes)
        if self.rec is not None:
            self.rec[q].append(lambda E=self.E[q], fn=fn, s=s: fn(E).then_inc(s, 16))
        else:
            fn(self.E[q]).then_inc(s, 16)
        self.dcnt[k] = n + 1
        tok = (s, 16 * (n + 1))
        self._commit(tok, reads, writes)
        return tok


class _Region:
    def __init__(self, cx, engines, thr):
        self.cx, self.engines, self.thr = cx, engines, thr

    def __enter__(self):
        cx = self.cx
        assert cx.rec is None
        self.c0 = dict(cx.ccnt)
        self.d0 = dict(cx.dcnt)
        self.w0 = {e: dict(d) for e, d in cx.waited.items()}
        cx.rec = {e: [] for e in cx.E}
        return self

    def __exit__(self, *a):
        cx = self.cx
        rec = cx.rec
        cx.rec = None
        for e in cx.E:
            if not rec[e]:
                continue
            assert e in self.engines, e
            E = cx.E[e]
            with E.If_cmp(cx.regs[e], self.thr, "IS_GE"):
                for t in rec[e]:
                    t()
            with E.Else():
                if e in cx.csem and cx.ccnt[e] > self.c0[e]:
                    if self.c0[e] > 0:
                        E.wait_ge(cx.csem[e], self.c0[e])
                    E.sem_inc(cx.csem[e], cx.ccnt[e] - self.c0[e])
                if e in cx.dsems:
                    for s_ in cx.dsems[e]:
                        k = id(s_)
                        n0, n1 = self.d0.get(k, 0), cx.dcnt.get(k, 0)
                        if n1 > n0:
                            if n0 > 0:
                                E.wait_ge(s_, 16 * n0)
                            E.sem_inc(s_, 16 * (n1 - n0))
                E.nop()
        cx.waited = self.w0
        return False


class Scope:
    def __init__(self, cx):
        self.cx = cx
        self.nc = cx.nc
        self.bufs = []
        self.stack = ExitStack()

    _n = [0]

    def sb(self, name, shape, dt):
        Scope._n[0] += 1
        name = "%s_%d" % (name, Scope._n[0])
        b = Buf(self.stack.enter_context(self.nc.sbuf_tensor(name, list(shape), dt)))
        self.bufs.append(b)
        return b

    def close(self):
        self.cx.release(self.bufs)
        self.stack.close()


def build(stage=99, dbg=False):
    nc = bass.Bass("TRN2", target_bir_lowering=False)
    cx = Ctx(nc)
    op, dma, peg = cx.op, cx.dma, cx.pe_group

    def din(name, shape, dt=F32):
        return Buf(nc.dram_tensor(name, list(shape), dt, kind="ExternalInput").ap())

    def dscr(name, shape, dt, out=False):
        kind = "ExternalOutput" if (out or dbg) else "Internal"
        return Buf(nc.dram_tensor(name, list(shape), dt, kind=kind).ap())

    def sb(name, shape, dt):
        return Buf(nc.alloc_sbuf_tensor(name, list(shape), dt))

    def psum(name, shape, dt=F32):
        b = Buf(nc.alloc_psum_tensor(name, list(shape), dt))
        b.excl = True
        return b

    xin = din("xin", [NALL, D])
    posb = din("posb", [NALL], I32)
    cvec = din("cvec", [P, KC])
    hmask_d = din("hmask", [P, 1])
    consts_d = din("consts", [P, C_END])
    ada_w = din("ada_w", [D, 6 * D])
    ada_b = din("ada_b", [6 * D])
    w_in = din("w_in", [D, 5632])
    hT_scr = dscr("hT_scr", [8, P, KC, 512], BF16)
    mod_scr = dscr("mod_scr", [6, D], F32)
    out_d = dscr("out", [NOWN, D], F32, out=True)

    cst = sb("cst", [P, C_END], F32)
    identb = sb("identb", [P, P], BF16)
    hmask = sb("hmask_s", [P, 1], F32)
    onesb = sb("onesb", [P, P], BF16)
    maskb = sb("maskb", [P, 256], BF16)
    maskh = sb("maskh", [P, P], BF16)
    pswapb = sb("pswapb", [P, P], BF16)
    slot_i = sb("slot_i", [P, 64], I32)
    gates = sb("gates", [P, 16, 4], F32)
    bchk = nc.gpsimd.alloc_register("bchk")
    nc.gpsimd.reg_mov(bchk, NSLOT - 1)
    idxs = [sb("idxs%d" % i, [P, 4], I32) for i in range(2)]
    nbt = sb("nbt", [1, NE], I32)
    nbf = sb("nbf", [1, NE], F32)
    scR = Scope(cx)
    cosT = scR.sb("cosT", [P, NALL], BF16)
    sinT = scR.sb("sinT", [P, NALL], BF16)
    scA = Scope(cx)
    sc1p = scA.sb("sc1p", [P, D], F32)
    sh1 = scA.sb("sh1", [P, D], F32)

    ps = [psum("ps%d" % i, [P, 512], F32) for i in range(8)]

    dma("sp", lambda q: q.dma_start(out=cst[:, :], in_=consts_d[:, :]), [consts_d], [cst])
    dma("sp", lambda q: q.dma_start(out=hmask[:, :], in_=hmask_d[:, :]), [hmask_d], [hmask])
    op("dve", lambda v: v.tensor_copy(identb[:, :], cst[:, C_ID:C_ID + 128]), [cst], [identb])

    if True:
        sc = Scope(cx)
        posi, ang, ang2 = sc.sb("posi", [P, NALL], I32), sc.sb("angf", [P, NALL], F32), sc.sb("ang2", [P, NALL], F32)
        dma("sp", lambda q: q.dma_start(out=posi[:, :], in_=posb.t.partition_broadcast(P)), [posb], [posi])
        op("dve", lambda v: v.tensor_copy(ang[:, :], posi[:, :]), [posi], [ang])
        op("dve", lambda v: v.tensor_scalar(ang[:, :], ang[:, :], cst[:, C_INVF:C_INVF + 1], None, ALU.mult),
           [ang, cst], [ang])
        op("dve", lambda v: v.tensor_scalar(ang2[:, :], ang[:, :], float(1.0 / TWO_PI), None, ALU.mult), [ang], [ang2])
        op("dve", lambda v: v.tensor_copy(posi[:, :], ang2[:, :]), [ang2], [posi])
        op("dve", lambda v: v.tensor_copy(ang2[:, :], posi[:, :]), [posi], [ang2])
        op("dve", lambda v: v.scalar_tensor_tensor(ang[:, :], ang2[:, :], -TWO_PI, ang[:, :], ALU.mult, ALU.add),
           [ang2, ang], [ang])
        op("dve", lambda v: v.tensor_scalar(ang2[:, :], ang[:, :], float(np.pi), -TWO_PI, ALU.is_gt, ALU.mult), [ang], [ang2])
        op("dve", lambda v: v.tensor_tensor(ang[:, :], ang[:, :], ang2[:, :], ALU.add), [ang, ang2], [ang])
        op("act", lambda a: a.activation(out=ang2[:, :], in_=ang[:, :], func=AF.Sin), [ang], [ang2])
        op("dve", lambda v: v.tensor_scalar(sinT[:, :], ang2[:, :], cst[:, C_SGN:C_SGN + 1], None, ALU.mult),
           [ang2, cst], [sinT])
        op("dve", lambda v: v.tensor_scalar_add(ang[:, :], ang[:, :], float(np.pi / 2)), [ang], [ang])
        op("dve", lambda v: v.tensor_scalar(ang2[:, :], ang[:, :], float(np.pi), -TWO_PI, ALU.is_gt, ALU.mult), [ang], [ang2])
        op("dve", lambda v: v.tensor_tensor(ang[:, :], ang[:, :], ang2[:, :], ALU.add), [ang, ang2], [ang])
        op("act", lambda a: a.activation(out=cosT[:, :], in_=ang[:, :], func=AF.Sin), [ang], [cosT])
        sc.close()

    if True:
        sc = Scope(cx)
        c_t, s_rep, abb, modt = sc.sb("c_t", [P, KC], F32), sc.sb("s_rep", [P, KC, P], F32), sc.sb("abb", [P, D], F32), sc.sb("modt", [P, D], F32)
        aw = [sc.sb("aw%d" % i, [P, D], F32) for i in range(3)]
        dma("sp", lambda q: q.dma_start(out=c_t[:, :], in_=cvec[:, :]), [cvec], [c_t])
        op("act", lambda a: a.activation(out=c_t[:, :], in_=c_t[:, :], func=AF.Silu), [c_t], [c_t])
        for kc in range(KC):
            op("dve", lambda v, kc=kc: v.tensor_copy(s_rep[:, kc, :], c_t[:, kc:kc + 1].to_broadcast([P, P])),
               [c_t], [s_rep])
        nld = 0
        for j in range(6):
            for kc in range(KC):
                b = aw[nld % 3]
                nld += 1
                dma("sp", lambda q, b=b, j=j, kc=kc: q.dma_start(out=b[:, :], in_=ada_w[kc * P:(kc + 1) * P, j * D:(j + 1) * D]),
                    [ada_w], [b])
                peg([lambda pe, b=b, n=n, kc=kc: pe.matmul(ps[n][:, :], s_rep[:, kc, :], b[:, n * 512:(n + 1) * 512],
                                                          start=(kc == 0), stop=(kc == KC - 1)) for n in range(4)],
                    [b, s_rep], [ps[0], ps[1], ps[2], ps[3]])
            dma("sp", lambda q, j=j: q.dma_start(out=abb[:, :], in_=ada_b.t[j * D:(j + 1) * D].partition_broadcast(P)),
                [ada_b], [abb])
            dst = sh1 if j == 0 else (sc1p if j == 1 else modt)
            for n in range(4):
                op("dve", lambda v, n=n, dst=dst: v.tensor_tensor(dst[:, n * 512:(n + 1) * 512], ps[n][:, :],
                                                                 abb[:, n * 512:(n + 1) * 512], ALU.add),
                   [ps[n], abb], [dst])
            if j == 1:
                op("dve", lambda v: v.tensor_scalar_add(sc1p[:, :], sc1p[:, :], 1.0), [sc1p], [sc1p])
            dma("sp", lambda q, j=j, dst=dst: q.dma_start(out=mod_scr[j:j + 1, :], in_=dst[0:1, :]), [dst], [mod_scr])
        sc.close()

    op("dve", lambda v: v.memset(onesb[:, :], 1.0), [], [onesb])
    op("dve", lambda v: v.tensor_copy(maskb[:, :], cst[:, C_MASK:C_MASK + 256]), [cst], [maskb])
    op("dve", lambda v: v.tensor_scalar(maskh[:, :], cst[:, C_MASK + 128:C_MASK + 256], hmask[:, 0:1], None, ALU.add),
       [cst, hmask], [maskh])
    op("dve", lambda v: v.tensor_copy(pswapb[:, :], cst[:, C_SWAP:C_SWAP + 128]), [cst], [pswapb])

    psn = [0]

    def nps():
        b = ps[psn[0] % 8]
        psn[0] += 1
        return b

    def bfview(pb, n=1024):
        return pb[:, :].bitcast(BF16)[:, 0:n]

    def ln_stats(src, st, mv, width=D):
        nchunk = width // 512
        for i in range(nchunk):
            op("dve", lambda v, i=i: v.bn_stats(st[:, i, :], src[:, i * 512:(i + 1) * 512]), [src], [st])
        op("dve", lambda v: v.bn_aggr(mv[:, 0:2], st[:, 0:nchunk, :]), [st], [mv])
        op("dve", lambda v: v.tensor_scalar_add(mv[:, 2:3], mv[:, 1:2], EPS), [mv], [mv])
        op("act", lambda a: a.activation(out=mv[:, 2:3], in_=mv[:, 2:3], func=AF.Sqrt), [mv], [mv])
        op("dve", lambda v: v.reciprocal(mv[:, 2:3], mv[:, 2:3]), [mv], [mv])
        op("dve", lambda v: v.tensor_scalar(mv[:, 3:4], mv[:, 0:1], mv[:, 2:3], -1.0, ALU.mult, ALU.mult), [mv], [mv])

    if True:
        sc = Scope(cx)
        xt = [sc.sb("xt%d" % i, [P, D], F32) for i in range(2)]
        hn = sc.sb("hn", [P, D], F32)
        hb = [sc.sb("hb%d" % i, [P, D], BF16) for i in range(2)]
        hTm = [sc.sb("hTm%d" % i, [P, KC, 512], BF16) for i in range(2)]
        st = sc.sb("st", [P, 4, 6], F32)
        mv = sc.sb("mv", [P, 4], F32)
        for lt in range(32):
            m, sub = lt // 4, lt % 4
            x_ = xt[lt % 2]
            h_ = hb[lt % 2]
            hT_ = hTm[m % 2]
            dma("sp", lambda q, x_=x_, lt=lt: q.dma_start(out=x_[:, :], in_=xin[lt * P:(lt + 1) * P, :]), [xin], [x_])
            ln_stats(x_, st, mv)
            op("act", lambda a, x_=x_: a.activation(out=hn[:, :], in_=x_[:, :], func=AF.Identity, bias=mv[:, 3:4], scale=mv[:, 2:3]),
               [x_, mv], [hn])
            op("dve", lambda g: g.tensor_tensor(hn[:, :], hn[:, :], sc1p[:, :], ALU.mult), [hn, sc1p], [hn])
            op("dve", lambda v, h_=h_: v.tensor_tensor(h_[:, :], hn[:, :], sh1[:, :], ALU.add), [hn, sh1], [h_])
            for half in range(2):
                pb = nps()
                peg([lambda pe, pb=pb, k=k, half=half, h_=h_: pe.transpose(bfview(pb)[:, k * P:(k + 1) * P],
                                                                        h_[:, (half * 8 + k) * P:(half * 8 + k + 1) * P], identb[:, :])
                     for k in range(8)], [h_, identb], [pb])
                op("act", lambda a, pb=pb, half=half, hT_=hT_, sub=sub: a.activation(
                    out=hT_[:, half * 8:(half + 1) * 8, sub * P:(sub + 1) * P],
                    in_=bfview(pb).rearrange("p (k t) -> p k t", t=P), func=AF.Identity), [pb], [hT_])
            if sub == 3:
                dma("sp", lambda q, hT_=hT_, m=m: q.dma_start(out=hT_scr[m, :, :, :], in_=hT_[:, :, :]), [hT_], [hT_scr])
        sc.close()
    scA.close()

    if stage == 1:
        cx.wait_all("sp", hT_scr)
        return nc

    wsT_d = din("wsT", [4, P, P])
    bspT_d = din("bspT", [P, 4])
    gln_g_d = din("gln_g", [512])
    gln_b_d = din("gln_b", [512])
    mixT_scr = dscr("mixT_scr", [4, P, 16, 512], BF16)
    w_in3 = w_in.t.rearrange("(kc p) n -> p kc n", p=P)

    numer_den_scope = None
    for g in range(NG):
        sc = Scope(cx)
        wg = sc.sb("wg", [P, KC, 3 * HG * P], BF16)
        QT = sc.sb("QT", [P, HG, NOWN], BF16)
        KT = sc.sb("KT", [P, HG, NALL], BF16)
        VT = sc.sb("VT", [P, HG, NALL], BF16)
        hTm = [sc.sb("hTg%d" % i, [P, KC, 512], BF16) for i in range(2)]
        rawb = [sc.sb("rawb%d" % i, [P, 512], BF16) for i in range(2)]
        t1 = [sc.sb("t1_%d" % i, [P, 512], F32) for i in range(2)]
        t2 = [sc.sb("t2_%d" % i, [P, 512], F32) for i in range(2)]
        W = HG * P
        for which in range(3):
            c0 = which * 1536 + g * W
            for kc in range(KC):
                dma("pool", lambda q, which=which, c0=c0, kc=kc: q.dma_start(out=wg[:, kc, which * W:(which + 1) * W],
                                                                              in_=w_in[kc * P:(kc + 1) * P, c0:c0 + W]), [w_in], [wg])
        un = 0
        for m in range(8):
            own = m >= 4
            hT_ = hTm[m % 2]
            dma("sp", lambda q, hT_=hT_, m=m: q.dma_start(out=hT_[:, :, :], in_=hT_scr[m, :, :, :]), [hT_scr], [hT_])
            tok0 = m * 512
            for hl in range(HG):
                for which in ((0, 1, 2) if own else (1, 2)):
                    if os.environ.get("KSKIP_QK") and which != 2:
                        continue
                    pb = nps()
                    cb = which * W + hl * P
                    peg([lambda pe, pb=pb, kc=kc, cb=cb, hT_=hT_: pe.matmul(pb[:, :], wg[:, kc, cb:cb + P], hT_[:, kc, :],
                                                                           start=(kc == 0), stop=(kc == KC - 1)) for kc in range(KC)],
                        [wg, hT_], [pb])
                    if which == 2:
                        op("act", lambda a, pb=pb, hl=hl, tok0=tok0: a.activation(out=VT[:, hl, tok0:tok0 + 512], in_=pb[:, :], func=AF.Identity),
                           [pb], [VT])
                        continue
                    rb, a1, a2 = rawb[un % 2], t1[un % 2], t2[un % 2]
                    un += 1
                    QKM = int(os.environ.get("QKM", "9"))
                    if which == 0:
                        dst, dl = QT, tok0 - NOWN
                    else:
                        dst, dl = KT, tok0
                    op("act", lambda a, pb=pb, rb=rb: a.activation(out=rb[:, :], in_=pb[:, :], func=AF.Identity), [pb], [rb])
                    if QKM == 1:
                        op("dve", lambda v, dst=dst, dl=dl, hl=hl, rb=rb: v.tensor_copy(dst[:, hl, dl:dl + 512], rb[:, :]), [rb], [dst])
                        continue
                    pb2 = nps()
                    peg([lambda pe, pb2=pb2, rb=rb: pe.matmul(pb2[:, :], pswapb[:, :], rb[:, :], start=True, stop=True)], [pswapb, rb], [pb2])
                    if QKM == 2:
                        op("dve", lambda v, dst=dst, dl=dl, hl=hl, pb2=pb2: v.tensor_copy(dst[:, hl, dl:dl + 512], pb2[:, :]), [pb2], [dst])
                        continue
                    op("dve", lambda v, pb=pb, a1=a1, tok0=tok0: v.tensor_tensor(a1[:, :], pb[:, :], cosT[:, tok0:tok0 + 512], ALU.mult),
                       [pb, cosT], [a1])
                    if QKM == 3:
                        op("dve", lambda v, dst=dst, dl=dl, hl=hl, a1=a1: v.tensor_copy(dst[:, hl, dl:dl + 512], a1[:, :]), [a1], [dst])
                        continue
                    op("dve", lambda v, pb2=pb2, a2=a2, tok0=tok0: v.tensor_tensor(a2[:, :], pb2[:, :], sinT[:, tok0:tok0 + 512], ALU.mult),
                       [pb2, sinT], [a2])
                    op("dve", lambda gp, dst=dst, dl=dl, hl=hl, a1=a1, a2=a2: gp.tensor_tensor(dst[:, hl, dl:dl + 512], a1[:, :], a2[:, :], ALU.add),
                       [a1, a2], [dst])

        if stage in (1.5, 2) and dbg and g == 0:
            dq = dscr("dbg_q", [P, HG, NOWN], BF16)
            dk = dscr("dbg_k", [P, HG, NALL], BF16)
            dv = dscr("dbg_v", [P, HG, NALL], BF16)
            dma("sp", lambda q: q.dma_start(out=dq[:, :, :], in_=QT[:, :, :]), [QT], [dq])
            dma("sp", lambda q: q.dma_start(out=dk[:, :, :], in_=KT[:, :, :]), [KT], [dk])
            dma("sp", lambda q: q.dma_start(out=dv[:, :, :], in_=VT[:, :, :]), [VT], [dv])
            if stage == 1.5:
                for b_ in (dq, dk, dv):
                    cx.wait_all("sp", b_)
                return nc

        sq = sc.sb("sq", [P, NALL], BF16)
        kmx = sc.sb("kmx", [P, 16], F32)
        negb = sc.sb("negb", [1, NOWN], BF16)
        qn = sc.sb("qn", [1, 512], F32)
        numer = sc.sb("numer", [P, NOWN], F32)
        den = sc.sb("den", [P, NOWN], F32)
        attb = sc.sb("attb", [P, NOWN], BF16)
        PT = [sc.sb("PT%d" % i, [P, 256], BF16) for i in range(4)]
        vblk = [sc.sb("vblk%d" % i, [P, P], BF16) for i in range(4)]
        bO = [ps[0], ps[1]]
        bL = [ps[2], ps[3]]
        bS = [ps[4], ps[5]]
        bV = ps[6]
        bX = ps[7]
        for hl in range(HG):
            h = g * HG + hl
            op("act", lambda a, hl=hl: a.activation(out=sq[:, :], in_=KT[:, hl, :], func=AF.Square), [KT], [sq])
            for c in range(8):
                peg([lambda pe, c=c: pe.matmul(bX[:, :], onesb[:, :], sq[:, c * 512:(c + 1) * 512], start=True, stop=True)], [onesb, sq], [bX])
                op("dve", lambda v, c=c: v.tensor_reduce(kmx[:, c:c + 1], bX[:, :], AX.X, ALU.max), [bX], [kmx])
            op("dve", lambda v: v.tensor_reduce(kmx[:, 8:9], kmx[:, 0:8], AX.X, ALU.max), [kmx], [kmx])
            op("act", lambda a: a.activation(out=kmx[:, 9:10], in_=kmx[:, 8:9], func=AF.Sqrt), [kmx], [kmx])
            op("dve", lambda v: v.tensor_scalar(kmx[:, 9:10], kmx[:, 9:10], -1.0, None, ALU.mult), [kmx], [kmx])
            op("act", lambda a, hl=hl: a.activation(out=sq[:, 0:NOWN], in_=QT[:, hl, :], func=AF.Square), [QT], [sq])
            for c in range(4):
                peg([lambda pe, c=c: pe.matmul(bX[:, :], onesb[:, :], sq[:, c * 512:(c + 1) * 512], start=True, stop=True)], [onesb, sq], [bX])
                op("act", lambda a: a.activation(out=qn[0:1, :], in_=bX[0:1, :], func=AF.Sqrt), [bX], [qn])
                op("dve", lambda v, c=c: v.tensor_scalar(negb[0:1, c * 512:(c + 1) * 512], qn[0:1, :], kmx[0:1, 9:10], None, ALU.mult),
                   [qn, kmx], [negb])
            first_branch = True
            sidx = 0
            for d in (1, 4, 16):
                nb_own = 16 // d
                n0 = 16 // d
                for r in range(d):
                    for n in range(n0 - 1, n0 + nb_own):
                        halo = (n == n0 - 1)
                        last = (n == n0 + nb_own - 1)
                        ks = n * P * d + r
                        kAP = KT[:, hl, sl(ks, P, d)]
                        vAP = VT[:, hl, sl(ks, P, d)]
                        if halo:
                            q0, nq = ks + P * d - NOWN, P
                            mAP = maskh[:, :]
                        elif last:
                            q0, nq = ks - NOWN, P
                            mAP = maskb[:, 0:P]
                        else:
                            q0, nq = ks - NOWN, 2 * P
                            mAP = maskb[:, :]
                        qAP = QT[:, hl, sl(q0, nq, d)]
                        nbAP = negb[0:1, sl(q0, nq, d)]
                        S = bS[sidx % 2]
                        pt = PT[sidx % 4]
                        vb = vblk[sidx % 4]
                        sidx += 1
                        peg([lambda pe, S=S, kAP=kAP, qAP=qAP, nq=nq: pe.matmul(S[:, 0:nq], kAP, qAP, start=True, stop=False),
                             lambda pe, S=S, mAP=mAP, nq=nq: pe.matmul(S[:, 0:nq], identb[:, :], mAP, start=False, stop=False),
                             lambda pe, S=S, nbAP=nbAP, nq=nq: pe.matmul(S[:, 0:nq], onesb[0:1, :], nbAP, start=False, stop=True)],
                            [KT, QT, identb, maskb, maskh, onesb, negb], [S])
                        op("act", lambda a, S=S, pt=pt, nq=nq: a.activation(out=pt[:, 0:nq], in_=S[:, 0:nq], func=AF.Exp, scale=SM_SCALE),
                           [S], [pt])
                        peg([lambda pe, vAP=vAP: pe.transpose(bfview(bV)[:, 0:P], vAP, identb[:, :])], [VT, identb], [bV])
                        op("dve", lambda v, vb=vb: v.tensor_copy(vb[:, :], bfview(bV)[:, 0:P]), [bV], [vb])
                        served = []
                        if halo:
                            served.append((n + 1, 0, True))
                        elif last:
                            served.append((n, 0, False))
                        else:
                            served.append((n, 0, False))
                            served.append((n + 1, P, True))
                        for (qb, co, isfirst) in served:
                            O = bO[qb % 2]
                            L = bL[qb % 2]
                            peg([lambda pe, O=O, vb=vb, pt=pt, co=co, isfirst=isfirst: pe.matmul(
                                O[:, 0:P], vb[:, :], pt[:, co:co + P], start=isfirst, stop=(not isfirst))], [vb, pt], [O])
                            peg([lambda pe, L=L, pt=pt, co=co, isfirst=isfirst: pe.matmul(
                                L[:, 0:P], onesb[:, :], pt[:, co:co + P], start=isfirst, stop=(not isfirst))], [onesb, pt], [L])
                            if not isfirst:
                                qs = qb * P * d + r - NOWN
                                nAP = numer[:, sl(qs, P, d)]
                                dAP = den[:, sl(qs, P, d)]
                                if first_branch:
                                    op("dve", lambda v, O=O, nAP=nAP: v.tensor_copy(nAP, O[:, 0:P]), [O], [numer])
                                    op("dve", lambda v, L=L, dAP=dAP: v.tensor_copy(dAP, L[:, 0:P]), [L], [den])
                                else:
                                    op("dve", lambda v, O=O, nAP=nAP: v.tensor_tensor(nAP, O[:, 0:P], nAP, ALU.add), [O, numer], [numer])
                                    op("dve", lambda v, L=L, dAP=dAP: v.tensor_tensor(dAP, L[:, 0:P], dAP, ALU.add), [L, den], [den])
                first_branch = False
            op("dve", lambda v: v.reciprocal(den[:, :], den[:, :]), [den], [den])
            op("dve", lambda v: v.tensor_tensor(attb[:, :], numer[:, :], den[:, :], ALU.mult), [numer, den], [attb])
            for mo in range(4):
                dma("sp", lambda q, h=h, mo=mo: q.dma_start(out=mixT_scr[mo, :, h, :], in_=attb[:, mo * 512:(mo + 1) * 512]), [attb], [mixT_scr])
        sc.close()
        if stage == 2 and dbg and g == 0:
            cx.wait_all("sp", mixT_scr)
            return nc

    scR.close()
    if True:
        sc = Scope(cx)
        wu = sc.sb("wu", [P, KC, 512], BF16)
        wv = sc.sb("wv", [P, KC, 512], BF16)
        wsT = sc.sb("wsT_s", [P, 4, P], F32)
        wsTb = sc.sb("wsTb", [P, 4, P], BF16)
        bsp = sc.sb("bsp", [P, 4], F32)
        glg = sc.sb("glg", [P, 512], F32)
        glb = sc.sb("glb", [P, 512], F32)
        hTm = [sc.sb("hTu%d" % i, [P, KC, 512], BF16) for i in range(2)]
        ug = [sc.sb("ug%d" % i, [P, 512], F32) for i in range(2)]
        vg = [sc.sb("vg%d" % i, [P, 512], F32) for i in range(2)]
        vnb = [sc.sb("vnb%d" % i, [P, 512], BF16) for i in range(2)]
        gm = [sc.sb("gm%d" % i, [P, 512], BF16) for i in range(2)]
        gmT = sc.sb("gmT", [P, 4, NOWN], BF16)
        st = sc.sb("stg", [P, 4, 6], F32)
        mv = sc.sb("mvg", [P, 4], F32)
        for kc in range(KC):
            dma("pool", lambda q, kc=kc: q.dma_start(out=wu[:, kc, :], in_=w_in[kc * P:(kc + 1) * P, 4608:5120]), [w_in], [wu])
            dma("pool", lambda q, kc=kc: q.dma_start(out=wv[:, kc, :], in_=w_in[kc * P:(kc + 1) * P, 5120:5632]), [w_in], [wv])
        dma("sp", lambda q: q.dma_start(out=wsT[:, :, :], in_=wsT_d.t.rearrange("g s t -> s g t")), [wsT_d], [wsT])
        dma("sp", lambda q: q.dma_start(out=bsp[:, :], in_=bspT_d[:, :]), [bspT_d], [bsp])
        dma("sp", lambda q: q.dma_start(out=glg[:, :], in_=gln_g_d.t.partition_broadcast(P)), [gln_g_d], [glg])
        dma("sp", lambda q: q.dma_start(out=glb[:, :], in_=gln_b_d.t.partition_broadcast(P)), [gln_b_d], [glb])
        for gg in range(4):
            op("dve", lambda v, gg=gg: v.tensor_tensor(wsTb[:, gg, :], wsT[:, gg, :], cst[:, C_TRIU:C_TRIU + P], ALU.mult), [wsT, cst], [wsTb])
        ti = 0
        for m in range(4, 8):
            hT_ = hTm[m % 2]
            dma("sp", lambda q, hT_=hT_, m=m: q.dma_start(out=hT_[:, :, :], in_=hT_scr[m, :, :, :]), [hT_scr], [hT_])
            for sub in range(4):
                ot = (m - 4) * 4 + sub
                u_, v_, vn_, gm_ = ug[ti % 2], vg[ti % 2], vnb[ti % 2], gm[ti % 2]
                ti += 1
                pu, pv = nps(), nps()
                peg([lambda pe, pu=pu, kc=kc, hT_=hT_, sub=sub: pe.matmul(pu[:, :], hT_[:, kc, sub * P:(sub + 1) * P], wu[:, kc, :],
                                                                        start=(kc == 0), stop=(kc == KC - 1)) for kc in range(KC)], [hT_, wu], [pu])
                peg([lambda pe, pv=pv, kc=kc, hT_=hT_, sub=sub: pe.matmul(pv[:, :], hT_[:, kc, sub * P:(sub + 1) * P], wv[:, kc, :],
                                                                        start=(kc == 0), stop=(kc == KC - 1)) for kc in range(KC)], [hT_, wv], [pv])
                op("act", lambda a, pu=pu, u_=u_: a.activation(out=u_[:, :], in_=pu[:, :], func=AF.Gelu), [pu], [u_])
                op("act", lambda a, pv=pv, v_=v_: a.activation(out=v_[:, :], in_=pv[:, :], func=AF.Gelu), [pv], [v_])
                ln_stats(v_, st, mv, width=512)
                op("act", lambda a, v_=v_: a.activation(out=v_[:, :], in_=v_[:, :], func=AF.Identity, bias=mv[:, 3:4], scale=mv[:, 2:3]),
                   [v_, mv], [v_])
                op("dve", lambda v, v_=v_: v.tensor_tensor(v_[:, :], v_[:, :], glg[:, :], ALU.mult), [v_, glg], [v_])
                op("dve", lambda v, v_=v_, vn_=vn_: v.tensor_tensor(vn_[:, :], v_[:, :], glb[:, :], ALU.add), [v_, glb], [vn_])
                pq = nps()
                peg([lambda pe, pq=pq, gg=gg, vn_=vn_: pe.matmul(pq[:, gg * P:(gg + 1) * P], wsTb[:, gg, :], vn_[:, gg * P:(gg + 1) * P],
                                                               start=True, stop=True) for gg in range(4)], [wsTb, vn_], [pq])
                for gg in range(4):
                    op("dve", lambda v, pq=pq, gg=gg, u_=u_, gm_=gm_: v.scalar_tensor_tensor(
                        gm_[:, gg * P:(gg + 1) * P], pq[:, gg * P:(gg + 1) * P], bsp[:, gg:gg + 1], u_[:, gg * P:(gg + 1) * P], ALU.add, ALU.mult),
                       [pq, bsp, u_], [gm_])
                pt_ = nps()
                peg([lambda pe, pt_=pt_, gg=gg, gm_=gm_: pe.transpose(bfview(pt_)[:, gg * P:(gg + 1) * P], gm_[:, gg * P:(gg + 1) * P], identb[:, :])
                     for gg in range(4)], [gm_, identb], [pt_])
                op("act", lambda a, pt_=pt_, ot=ot: a.activation(out=gmT[:, :, ot * P:(ot + 1) * P],
                                                                in_=bfview(pt_, 512).rearrange("p (k t) -> p k t", t=P), func=AF.Identity),
                   [pt_], [gmT])
        for mo in range(4):
            dma("sp", lambda q, mo=mo: q.dma_start(out=mixT_scr[mo, :, 12:16, :], in_=gmT[:, :, mo * 512:(mo + 1) * 512]), [gmT], [mixT_scr])
        sc.close()

    if stage == 2:
        cx.wait_all("sp", mixT_scr)
        return nc

    w_out_d = din("w_out", [D, D])
    ln1_g_d = din("ln1_g", [D])
    ln1_b_d = din("ln1_b", [D])
    router_w_d = din("router_w", [D, NE])
    router_b_d = din("router_b", [NE])
    x1_scr = dscr("x1_scr", [NOWN, D], F32)
    Xs = dscr("Xs", [NSLOT, D], BF16)
    if True:
        sc = Scope(cx)
        wob = sc.sb("wob", [P, KC, D], BF16)
        g1bc = sc.sb("g1bc", [P, D], F32)
        l1g = sc.sb("l1g", [P, D], F32)
        l1b = sc.sb("l1b", [P, D], F32)
        sc2p = sc.sb("sc2p", [P, D], F32)
        sh2b = sc.sb("sh2b", [P, D], F32)
        mixTm = [sc.sb("mixTm%d" % i, [P, KC, 512], BF16) for i in range(2)]
        xt = [sc.sb("xt3_%d" % i, [P, D], F32) for i in range(2)]
        y1 = sc.sb("y1", [P, D], F32)
        x1t = sc.sb("x1t", [P, D], F32)
        h2b = sc.sb("h2b", [P, D], BF16)
        h2T = sc.sb("h2T", [P, KC, P], F32)
        rw = sc.sb("rw", [P, KC, NE], F32)
        rbb = sc.sb("rbb", [P, NE], F32)
        lg = sc.sb("lg", [P, NE], F32)
        m8 = sc.sb("m8", [P, 8], F32)
        sm = sc.sb("sm", [P, 8], F32)
        mkf = sc.sb("mkf", [P, NE], F32)
        mkb = sc.sb("mkb", [P, NE], BF16)
        ustrb = sc.sb("ustrb", [P, P], BF16)
        cntp = sc.sb("cntp", [P, NE], F32)
        slotf = sc.sb("slotf", [P, NE], F32)
        prod = sc.sb("prod", [P, NE], F32)
        slot4 = sc.sb("slot4", [P, 4], F32)
        st = sc.sb("st3", [P, 4, 6], F32)
        mv = sc.sb("mv3", [P, 4], F32)
        for kc in range(KC):
            dma("pool", lambda q, kc=kc: q.dma_start(out=wob[:, kc, :], in_=w_out_d[kc * P:(kc + 1) * P, :]), [w_out_d], [wob])
        dma("sp", lambda q: q.dma_start(out=g1bc[:, :], in_=mod_scr.t[2, :].partition_broadcast(P)), [mod_scr], [g1bc])
        dma("sp", lambda q: q.dma_start(out=sh2b[:, :], in_=mod_scr.t[3, :].partition_broadcast(P)), [mod_scr], [sh2b])
        dma("sp", lambda q: q.dma_start(out=sc2p[:, :], in_=mod_scr.t[4, :].partition_broadcast(P)), [mod_scr], [sc2p])
        dma("sp", lambda q: q.dma_start(out=l1g[:, :], in_=ln1_g_d.t.partition_broadcast(P)), [ln1_g_d], [l1g])
        dma("sp", lambda q: q.dma_start(out=l1b[:, :], in_=ln1_b_d.t.partition_broadcast(P)), [ln1_b_d], [l1b])
        dma("sp", lambda q: q.dma_start(out=rw[:, :, :], in_=router_w_d.t.rearrange("(kc p) e -> p kc e", p=P)), [router_w_d], [rw])
        dma("sp", lambda q: q.dma_start(out=rbb[:, :], in_=router_b_d.t.partition_broadcast(P)), [router_b_d], [rbb])
        op("dve", lambda v: v.tensor_scalar_add(sc2p[:, :], sc2p[:, :], 1.0), [sc2p], [sc2p])
        op("dve", lambda v: v.tensor_copy(ustrb[:, :], cst[:, C_USTR:C_USTR + P]), [cst], [ustrb])
        op("dve", lambda v: v.memset(cntp[:, :], 0.0), [], [cntp])
        for ot in range(16):
            m, sub = ot // 4, ot % 4
            mt_ = mixTm[m % 2]
            x_ = xt[ot % 2]
            if sub == 0:
                dma("sp", lambda q, mt_=mt_, m=m: q.dma_start(out=mt_[:, :, :], in_=mixT_scr[m, :, :, :]), [mixT_scr], [mt_])
            dma("sp", lambda q, x_=x_, ot=ot: q.dma_start(out=x_[:, :], in_=xin[NOWN + ot * P:NOWN + (ot + 1) * P, :]), [xin], [x_])
            for n in range(4):
                pb = nps()
                peg([lambda pe, pb=pb, c=c, n=n, mt_=mt_, sub=sub: pe.matmul(pb[:, :], mt_[:, c, sub * P:(sub + 1) * P], wob[:, c, n * 512:(n + 1) * 512],
                                                                          start=(c == 0), stop=(c == KC - 1)) for c in range(KC)], [mt_, wob], [pb])
                op("dve", lambda v, pb=pb, n=n: v.tensor_tensor(y1[:, n * 512:(n + 1) * 512], pb[:, :], g1bc[:, n * 512:(n + 1) * 512], ALU.mult),
                   [pb, g1bc], [y1])
            op("dve", lambda gp, x_=x_: gp.scalar_tensor_tensor(y1[:, :], x_[:, :], ALPHA, y1[:, :], ALU.mult, ALU.add), [x_, y1], [y1])
            ln_stats(y1, st, mv)
            op("act", lambda a: a.activation(out=x1t[:, :], in_=y1[:, :], func=AF.Identity, bias=mv[:, 3:4], scale=mv[:, 2:3]), [y1, mv], [x1t])
            op("dve", lambda v: v.tensor_tensor(x1t[:, :], x1t[:, :], l1g[:, :], ALU.mult), [x1t, l1g], [x1t])
            op("dve", lambda gp: gp.tensor_tensor(x1t[:, :], x1t[:, :], l1b[:, :], ALU.add), [x1t, l1b], [x1t])
            dma("sp", lambda q, ot=ot: q.dma_start(out=x1_scr[ot * P:(ot + 1) * P, :], in_=x1t[:, :]), [x1t], [x1_scr])
            ln_stats(x1t, st, mv)
            op("act", lambda a: a.activation(out=y1[:, :], in_=x1t[:, :], func=AF.Identity, bias=mv[:, 3:4], scale=mv[:, 2:3]), [x1t, mv], [y1])
            op("dve", lambda v: v.tensor_tensor(y1[:, :], y1[:, :], sc2p[:, :], ALU.mult), [y1, sc2p], [y1])
            op("dve", lambda gp: gp.tensor_tensor(y1[:, :], y1[:, :], sh2b[:, :], ALU.add), [y1, sh2b], [y1])
            op("act", lambda a: a.activation(out=h2b[:, :], in_=y1[:, :], func=AF.Identity), [y1], [h2b])
            for qd in range(4):
                pb = nps()
                peg([lambda pe, pb=pb, k=k, qd=qd: pe.transpose(pb[:, k * P:(k + 1) * P], y1[:, (qd * 4 + k) * P:(qd * 4 + k + 1) * P], cst[:, C_ID:C_ID + P])
                     for k in range(4)], [y1, cst], [pb])
                op("act", lambda a, pb=pb, qd=qd: a.activation(out=h2T[:, qd * 4:(qd + 1) * 4, :], in_=pb[:, :].rearrange("p (k t) -> p k t", t=P),
                                                            func=AF.Identity), [pb], [h2T])
            pl = nps()
            peg([lambda pe, pl=pl, kc=kc: pe.matmul(pl[:, 0:NE], h2T[:, kc, :], rw[:, kc, :], start=(kc == 0), stop=(kc == KC - 1)) for kc in range(KC)],
                [h2T, rw], [pl])
            op("dve", lambda v, pl=pl: v.tensor_tensor(lg[:, :], pl[:, 0:NE], rbb[:, :], ALU.add), [pl, rbb], [lg])
            op("dve", lambda v: v.max(m8[:, :], lg[:, :]), [lg], [m8])
            op("dve", lambda v: v.tensor_scalar(sm[:, 0:1], m8[:, 0:1], -1.0, None, ALU.mult), [m8], [sm])
            op("act", lambda a: a.activation(out=sm[:, 4:8], in_=m8[:, 0:4], func=AF.Exp, bias=sm[:, 0:1], scale=1.0), [m8, sm], [sm])
            op("dve", lambda v: v.tensor_reduce(sm[:, 1:2], sm[:, 4:8], AX.X, ALU.add), [sm], [sm])
            op("dve", lambda v: v.reciprocal(sm[:, 2:3], sm[:, 1:2]), [sm], [sm])
            op("dve", lambda v, ot=ot: v.tensor_scalar(gates[:, ot, :], sm[:, 4:8], sm[:, 2:3], None, ALU.mult), [sm], [gates])
            op("dve", lambda v: v.tensor_scalar(mkf[:, :], lg[:, :], m8[:, 3:4], None, ALU.is_ge), [lg, m8], [mkf])
            op("dve", lambda v: v.tensor_copy(mkb[:, :], mkf[:, :]), [mkf], [mkb])
            pr = nps()
            peg([lambda pe, pr=pr: pe.matmul(pr[:, 0:NE], ustrb[:, :], mkb[:, :], start=True, stop=True),
                 lambda pe, pr=pr: pe.matmul(pr[:, NE:2 * NE], onesb[:, :], mkb[:, :], start=True, stop=True)], [ustrb, onesb, mkb], [pr])
            op("dve", lambda v, pr=pr: v.tensor_tensor(slotf[:, :], pr[:, 0:NE], cntp[:, :], ALU.add), [pr, cntp], [slotf])
            op("dve", lambda v: v.tensor_scalar(prod[:, :], slotf[:, :], float(CAP), 1.0e6, ALU.is_ge, ALU.mult), [slotf], [prod])
            op("dve", lambda v: v.tensor_tensor(slotf[:, :], slotf[:, :], cst[:, C_EOFF:C_EOFF + NE], ALU.add), [slotf, cst], [slotf])
            op("dve", lambda v: v.tensor_tensor(slotf[:, :], slotf[:, :], prod[:, :], ALU.add), [slotf, prod], [slotf])
            op("dve", lambda v, pr=pr: v.tensor_tensor(cntp[:, :], pr[:, NE:2 * NE], cntp[:, :], ALU.add), [pr, cntp], [cntp])
            for k in range(4):
                op("dve", lambda v, k=k: v.scalar_tensor_tensor(prod[:, :], lg[:, :], m8[:, k:k + 1], slotf[:, :], ALU.is_equal, ALU.mult),
                   [lg, m8, slotf], [prod])
                op("dve", lambda v, k=k: v.tensor_reduce(slot4[:, k:k + 1], prod[:, :], AX.X, ALU.add), [prod], [slot4])
            op("dve", lambda v, ot=ot: v.tensor_copy(slot_i[:, ot * 4:ot * 4 + 4], slot4[:, :]), [slot4], [slot_i])
            op("dve", lambda v: v.tensor_scalar(sm[:, 4:8], slot4[:, :], float(NSLOT), None, ALU.is_lt), [slot4], [sm])
            op("dve", lambda v, ot=ot: v.tensor_tensor(gates[:, ot, :], gates[:, ot, :], sm[:, 4:8], ALU.mult), [gates, sm], [gates])
            ix = idxs[ot % 2]
            op("dve", lambda v, ix=ix: v.tensor_copy(ix[:, :], slot4[:, :]), [slot4], [ix])
            for k in range(4):
                dma("pool", lambda q, ix=ix, k=k: q.indirect_dma_start(
                    out=Xs[:, :], out_offset=bass.IndirectOffsetOnAxis(ap=ix[:, k:k + 1], axis=0),
                    in_=h2b[:, :], in_offset=None, bounds_check=bchk, oob_is_err=False), [h2b, ix], [Xs])
        op("dve", lambda v: v.tensor_scalar(nbf[0:1, :], cntp[0:1, :], 127.0, 1.0 / 128.0, ALU.add, ALU.mult), [cntp], [nbf])
        op("dve", lambda v: v.tensor_scalar(nbf[0:1, :], nbf[0:1, :], -0.496, float(CAP // P), ALU.add, ALU.min), [nbf], [nbf])
        op("dve", lambda v: v.tensor_copy(nbt[0:1, :], nbf[0:1, :]), [nbf], [nbt])
        sc.close()

    if stage == 3:
        dbg3 = dscr("dbg3", [P, 16, 8], F32, out=True)
        if True:
            sc = Scope(cx)
            tmp = sc.sb("dbgtmp", [P, 16, 8], F32)
            op("dve", lambda v: v.tensor_copy(tmp[:, :, 0:4], slot_i[:, :].rearrange("p (t k) -> p t k", k=4)), [slot_i], [tmp])
            op("dve", lambda v: v.tensor_copy(tmp[:, :, 4:8], gates[:, :, :]), [gates], [tmp])
            dma("sp", lambda q: q.dma_start(out=dbg3[:, :, :], in_=tmp[:, :, :]), [tmp], [dbg3])
            cx.wait_all("sp", dbg3)
            cx.wait_all("sp", x1_scr)
            cx.wait_all("sp", Xs)
            sc.close()
        return nc

    w_gu_d = din("w_gu", [NE, D, 2 * D])
    w_d_d = din("w_d", [NE, D, D])
    bguT_d = din("bguT", [NE, P, 32])
    b_d_d = din("b_d", [NE, D])
    ln2_g_d = din("ln2_g", [D])
    ln2_b_d = din("ln2_b", [D])
    Y = dscr("Y", [NSLOT, D], F32)
    NSB = CAP // P
    NHALF = CAP // 512
    if True:
        sc = Scope(cx)
        stg = [sc.sb("stg%d" % i, [P, 8, 512], F32) for i in range(3)]
        wbf = [sc.sb("wbf%d" % i, [P, KC, 512], BF16) for i in range(3)]
        xrow = [sc.sb("xrow%d" % i, [P, D], BF16) for i in range(2)]
        xT = sc.sb("xT", [P, KC, CAP], BF16)
        actT = sc.sb("actT", [P, KC, CAP], BF16)
        yst = [sc.sb("yst%d" % i, [P, 512], F32) for i in range(4)]
        bdbc = [sc.sb("bdbc%d" % i, [P, D], BF16) for i in range(2)]
        bgu = [sc.sb("bgu%d" % i, [P, 32], F32) for i in range(2)]
        gt = [sc.sb("gt%d" % i, [P, 256], F32) for i in range(4)]
        ut = [sc.sb("ut%d" % i, [P, 256], F32) for i in range(4)]
        sg = [sc.sb("sg%d" % i, [P, 256], F32) for i in range(4)]
        NW = len(wbf)

        gran = []
        for e in range(NE):
            for q4 in range(4):
                gran.append((e, 0, q4))
                gran.append((e, 1, q4))
            for n in range(4):
                gran.append((e, 2, n))
        emitted = [0]
        cast_rot = ["dve", "act", "dve"]
        hcount = [0]

        def emit_load(i):
            e, kind, idx = gran[i]
            wb = wbf[i % NW]
            for half in range(2):
                s_ = stg[hcount[0] % len(stg)]
                eng = cast_rot[hcount[0] % len(cast_rot)]
                hcount[0] += 1
                if kind == 2:
                    src = w_d_d.t[e].rearrange("(kc p) n -> p kc n", p=P)[:, half * 8:(half + 1) * 8, idx * 512:(idx + 1) * 512]
                    dep = w_d_d
                else:
                    c0 = kind * D + idx * 512
                    src = w_gu_d.t[e].rearrange("(kc p) n -> p kc n", p=P)[:, half * 8:(half + 1) * 8, c0:c0 + 512]
                    dep = w_gu_d
                dma("sp", lambda q, s_=s_, src=src: q.dma_start(out=s_[:, :, :], in_=src), [dep], [s_])
                if eng == "act":
                    op("act", lambda a, wb=wb, half=half, s_=s_: a.copy(out=wb[:, half * 8:(half + 1) * 8, :], in_=s_[:, :, :]),
                       [s_], [wb])
                else:
                    op(eng, lambda v, wb=wb, half=half, s_=s_: v.tensor_copy(wb[:, half * 8:(half + 1) * 8, :], s_[:, :, :]), [s_], [wb])

        def ensure(upto):
            while emitted[0] < min(upto, len(gran)):
                emit_load(emitted[0])
                emitted[0] += 1

        for en in ("pe", "act", "dve", "pool", "sp"):
            cx.regs[en] = cx.E[en].alloc_register("nb_" + en)
        gi = 0
        ensure(2)
        cnt2 = 0
        ycnt = 0
        for e in range(NE):
            bg_, bd_ = bgu[e % 2], bdbc[e % 2]
            for en in ("pe", "act", "dve", "pool", "sp"):
                cx.wait_all(en, nbt)
                cx.E[en].reg_load(cx.regs[en], nbt[0:1, e:e + 1])
            dma("sp", lambda q, e=e, bg_=bg_: q.dma_start(out=bg_[:, :], in_=bguT_d[e, :, :]), [bguT_d], [bg_])
            dma("pool", lambda q, e=e, bd_=bd_: q.dma_start(out=bd_[:, :], in_=b_d_d.t[e, :].partition_broadcast(P)), [b_d_d], [bd_])
            for sbk in range(NSB):
                with cx.region(("pe", "act"), sbk + 1):
                    xr = xrow[(e * NSB + sbk) % 2]
                    r0 = e * CAP + sbk * P
                    dma("act", lambda q, xr=xr, r0=r0: q.dma_start(out=xr[:, :], in_=Xs[r0:r0 + P, :]), [Xs], [xr])
                    for half in range(2):
                        pb = nps()
                        peg([lambda pe, pb=pb, k=k, half=half, xr=xr: pe.transpose(bfview(pb)[:, k * P:(k + 1) * P],
                                                                                 xr[:, (half * 8 + k) * P:(half * 8 + k + 1) * P], identb[:, :])
                             for k in range(8)], [xr, identb], [pb])
                        op("act", lambda a, pb=pb, half=half, sbk=sbk: a.copy(
                            out=xT[:, half * 8:(half + 1) * 8, sbk * P:(sbk + 1) * P],
                            in_=bfview(pb).rearrange("p (k t) -> p k t", t=P)), [pb], [xT])
            for q4 in range(4):
                ensure(gi + 3)
                wg_, wu_ = wbf[gi % NW], wbf[(gi + 1) % NW]
                for jj in range(4):
                    j = q4 * 4 + jj
                    for qt in range(CAP // 256):
                        with cx.region(("pe", "dve", "pool", "act"), 2 * qt + 1):
                            g_, u_, s_g = gt[cnt2 % 4], ut[cnt2 % 4], sg[cnt2 % 4]
                            cnt2 += 1
                            pg, pu = nps(), nps()
                            c0 = qt * 256
                            peg([lambda pe, pg=pg, kc=kc, jj=jj, wg_=wg_, c0=c0: pe.matmul(pg[:, 0:256], wg_[:, kc, jj * P:(jj + 1) * P], xT[:, kc, c0:c0 + 256],
                                                                                          start=(kc == 0), stop=(kc == KC - 1)) for kc in range(KC)], [wg_, xT], [pg])
                            peg([lambda pe, pu=pu, kc=kc, jj=jj, wu_=wu_, c0=c0: pe.matmul(pu[:, 0:256], wu_[:, kc, jj * P:(jj + 1) * P], xT[:, kc, c0:c0 + 256],
                                                                                          start=(kc == 0), stop=(kc == KC - 1)) for kc in range(KC)], [wu_, xT], [pu])
                            op("dve", lambda v, pg=pg, g_=g_, j=j, bg_=bg_: v.tensor_scalar(g_[:, 0:256], pg[:, 0:256], bg_[:, j:j + 1], 7.0, ALU.add, ALU.min),
                               [pg, bg_], [g_])
                            op("dve", lambda v, pu=pu, u_=u_, j=j, bg_=bg_: v.tensor_scalar(u_[:, 0:256], pu[:, 0:256], bg_[:, 16 + j:17 + j], 7.0, ALU.add, ALU.min),
                               [pu, bg_], [u_])
                            op("dve", lambda gp, u_=u_: gp.tensor_scalar(u_[:, 0:256], u_[:, 0:256], -7.0, 1.0, ALU.max, ALU.add), [u_], [u_])
                            op("act", lambda a, g_=g_, s_g=s_g: a.activation(out=s_g[:, 0:256], in_=g_[:, 0:256], func=AF.Sigmoid, scale=1.702), [g_], [s_g])
                            op("pool", lambda gp, g_=g_, s_g=s_g: gp.tensor_tensor(g_[:, 0:256], g_[:, 0:256], s_g[:, 0:256], ALU.mult), [g_, s_g], [g_])
                            op("pool", lambda v, g_=g_, u_=u_, j=j, c0=c0: v.tensor_tensor(actT[:, j, c0:c0 + 256], g_[:, 0:256], u_[:, 0:256], ALU.mult), [g_, u_], [actT])
                gi += 2
            for n in range(4):
                ensure(gi + 2)
                wd_ = wbf[gi % NW]
                for sbk in range(NSB):
                    with cx.region(("pe", "dve", "act"), sbk + 1):
                        ys = yst[ycnt % len(yst)]
                        ycnt += 1
                        pb = nps()
                        peg([lambda pe, pb=pb, j=j, sbk=sbk, wd_=wd_: pe.matmul(pb[:, :], actT[:, j, sbk * P:(sbk + 1) * P], wd_[:, j, :],
                                                                               start=(j == 0), stop=(j == KC - 1)) for j in range(KC)], [actT, wd_], [pb])
                        op("dve", lambda v, pb=pb, ys=ys, n=n, bd_=bd_: v.tensor_tensor(ys[:, :], pb[:, :], bd_[:, n * 512:(n + 1) * 512], ALU.add),
                           [pb, bd_], [ys])
                        r0 = e * CAP + sbk * P
                        dma("act", lambda q, ys=ys, r0=r0, n=n: q.dma_start(out=Y[r0:r0 + P, n * 512:(n + 1) * 512], in_=ys[:, :]), [ys], [Y])
                gi += 1
        sc.close()

    if True:
        sc = Scope(cx)
        g2bc = sc.sb("g2bc", [P, D], F32)
        l2g = sc.sb("l2g", [P, D], F32)
        l2b = sc.sb("l2b", [P, D], F32)
        yk = [sc.sb("yk%d" % i, [P, D], F32) for i in range(4)]
        x1r = [sc.sb("x1r%d" % i, [P, D], F32) for i in range(2)]
        acc = sc.sb("acc", [P, D], F32)
        ot_ = [sc.sb("outt%d" % i, [P, D], F32) for i in range(2)]
        st = sc.sb("st4", [P, 4, 6], F32)
        mv = sc.sb("mv4", [P, 4], F32)
        for k in range(4):
            op("dve", lambda v, k=k: v.memset(yk[k][:, :], 0.0), [], [yk[k]])
        dma("sp", lambda q: q.dma_start(out=g2bc[:, :], in_=mod_scr.t[5, :].partition_broadcast(P)), [mod_scr], [g2bc])
        dma("sp", lambda q: q.dma_start(out=l2g[:, :], in_=ln2_g_d.t.partition_broadcast(P)), [ln2_g_d], [l2g])
        dma("sp", lambda q: q.dma_start(out=l2b[:, :], in_=ln2_b_d.t.partition_broadcast(P)), [ln2_b_d], [l2b])
        for ot in range(16):
            x1_ = x1r[ot % 2]
            o_ = ot_[ot % 2]
            dma("sp", lambda q, x1_=x1_, ot=ot: q.dma_start(out=x1_[:, :], in_=x1_scr[ot * P:(ot + 1) * P, :]), [x1_scr], [x1_])
            ix = idxs[ot % 2]
            op("dve", lambda v, ix=ix, ot=ot: v.tensor_copy(ix[:, :], slot_i[:, ot * 4:ot * 4 + 4]), [slot_i], [ix])
            for k in range(4):
                dma("pool", lambda q, ix=ix, k=k: q.indirect_dma_start(
                    out=yk[k][:, :], out_offset=None, in_=Y[:, :],
                    in_offset=bass.IndirectOffsetOnAxis(ap=ix[:, k:k + 1], axis=0), bounds_check=bchk, oob_is_err=False),
                    [Y, ix], [yk[k]])
            op("dve", lambda v, ot=ot: v.tensor_scalar(acc[:, :], yk[0][:, :], gates[:, ot, 0:1], None, ALU.mult), [yk[0], gates], [acc])
            for k in range(1, 4):
                op("dve", lambda v, ot=ot, k=k: v.scalar_tensor_tensor(acc[:, :], yk[k][:, :], gates[:, ot, k:k + 1], acc[:, :], ALU.mult, ALU.add),
                   [yk[k], gates, acc], [acc])
            op("dve", lambda gp: gp.tensor_tensor(acc[:, :], acc[:, :], g2bc[:, :], ALU.mult), [acc, g2bc], [acc])
            op("dve", lambda v, x1_=x1_: v.scalar_tensor_tensor(acc[:, :], x1_[:, :], ALPHA, acc[:, :], ALU.mult, ALU.add), [x1_, acc], [acc])
            ln_stats(acc, st, mv)
            op("act", lambda a, o_=o_: a.activation(out=o_[:, :], in_=acc[:, :], func=AF.Identity, bias=mv[:, 3:4], scale=mv[:, 2:3]), [acc, mv], [o_])
            op("dve", lambda gp, o_=o_: gp.tensor_tensor(o_[:, :], o_[:, :], l2g[:, :], ALU.mult), [o_, l2g], [o_])
            op("dve", lambda v, o_=o_: v.tensor_tensor(o_[:, :], o_[:, :], l2b[:, :], ALU.add), [o_, l2b], [o_])
            dma("sp", lambda q, o_=o_, ot=ot: q.dma_start(out=out_d[ot * P:(ot + 1) * P, :], in_=o_[:, :]), [o_], [out_d])
        cx.wait_all("sp", out_d)
        sc.close()
    return nc


def make_in_maps(inputs, stage=99):
    x = np.asarray(inputs["x"], np.float32)
    c = np.asarray(inputs["c"], np.float32)
    pos = np.asarray(inputs["positions"], np.int32)
    consts = make_consts()
    maps = []
    for core in range(8):
        b, half = core // 2, core % 2
        m = {}
        if half == 0:
            xin = np.concatenate([np.zeros((NOWN, D), np.float32), x[b, 0:NOWN]], axis=0)
            pp = np.concatenate([np.zeros((NOWN,), np.int32), pos[b, 0:NOWN]])
            hm = np.full((P, 1), -BIG, np.float32)
        else:
            xin = x[b]
            pp = pos[b]
            hm = np.zeros((P, 1), np.float32)
        m["xin"] = np.ascontiguousarray(xin)
        m["posb"] = np.ascontiguousarray(pp)
        m["cvec"] = np.ascontiguousarray(c[b].reshape(KC, P).T)
        m["hmask"] = hm
        m["consts"] = consts
        m["ada_w"] = np.asarray(inputs["ada_w"], np.float32)[0]
        m["ada_b"] = np.asarray(inputs["ada_b"], np.float32)[0]
        m["w_in"] = np.asarray(inputs["w_in"], np.float32)[0]
        if stage >= 2:
            m["wsT"] = np.ascontiguousarray(np.asarray(inputs["w_spatial"], np.float32)[0].transpose(0, 2, 1))
            m["bspT"] = np.ascontiguousarray(np.asarray(inputs["b_spatial"], np.float32)[0].T)
            m["gln_g"] = np.asarray(inputs["gmlp_ln_g"], np.float32)[0]
            m["gln_b"] = np.asarray(inputs["gmlp_ln_b"], np.float32)[0]
        if stage >= 3:
            m["w_out"] = np.asarray(inputs["w_out"], np.float32)[0]
            m["ln1_g"] = np.asarray(inputs["ln1_g"], np.float32)[0]
            m["ln1_b"] = np.asarray(inputs["ln1_b"], np.float32)[0]
            m["router_w"] = np.asarray(inputs["router_w"], np.float32)[0]
            m["router_b"] = np.asarray(inputs["router_b"], np.float32)[0]
        if stage >= 4:
            m["w_gu"] = np.asarray(inputs["w_gate_up"], np.float32)[0]
            m["w_d"] = np.asarray(inputs["w_down"], np.float32)[0]
            bgu = np.asarray(inputs["b_gate_up"], np.float32)[0]
            m["bguT"] = np.ascontiguousarray(bgu.reshape(NE, 32, P).transpose(0, 2, 1))
            m["b_d"] = np.asarray(inputs["b_down"], np.float32)[0]
            m["ln2_g"] = np.asarray(inputs["ln2_g"], np.float32)[0]
            m["ln2_b"] = np.asarray(inputs["ln2_b"], np.float32)[0]
        maps.append(m)
    return maps


def kernel(**inputs):
    nc = build()
    maps = make_in_maps(inputs)
    res = run_bass_kernel_spmd(nc, maps, core_ids=list(range(8)))
    out = np.zeros((4, 4096, D), np.float32)
    for core in range(8):
        b, half = core // 2, core % 2
        out[b, half * NOWN:(half + 1) * NOWN] = res.results[core]["out"]
    return out
```
